# Optimizing a Trainium2 kernel written in Bass

```python
import math
import jax, jax.numpy as jnp
from jax import lax
import numpy as np

D_MODEL = 2048
BATCH = 1
SEQ = 8192
DEPTH = 4

GRID_W = 64
CTX_LEN = 256
MIX_W = D_MODEL
S5_W = D_MODEL // 4
S5_CH = 16
S5_G = S5_W // S5_CH
S5_P = 64
CONV_W = D_MODEL // 4
CONV_K = 31
ML_W = D_MODEL // 2
ML_H = 4
ML_DH = ML_W // ML_H
ML_CHUNK = 128
D_FF = ((8 * D_MODEL + 3 * 256 - 1) // (3 * 256)) * 256
EPS = 1e-6
LN_EPS = 1e-5

OFF_S5 = 0
OFF_CONV = OFF_S5 + S5_W
OFF_Q = OFF_CONV + 2 * CONV_W
OFF_K = OFF_Q + ML_W
OFF_V = OFF_K + ML_W
OFF_O = OFF_V + ML_W
OFF_G = OFF_O + ML_W
IN_W = OFF_G + 4 * ML_H

kernel_name = 'hymba_s5_conformer_mlstm_dit'


def _rmsnorm(x, g):
    xf = x.astype(jnp.float32)
    xf = xf * lax.rsqrt(jnp.mean(xf * xf, axis=-1, keepdims=True) + EPS)
    return (xf * g.astype(jnp.float32)).astype(x.dtype)


def _layernorm(x, g, b):
    xf = x.astype(jnp.float32)
    xc = xf - jnp.mean(xf, axis=-1, keepdims=True)
    var = jnp.mean(xc * xc, axis=-1, keepdims=True)
    return (xc * lax.rsqrt(var + LN_EPS) * g.astype(jnp.float32) + b.astype(jnp.float32)).astype(x.dtype)


def _adaln(c_vec, w_mod, b_mod):
    m = jax.nn.silu(c_vec) @ w_mod + b_mod
    return jnp.split(m[..., None, :], 6, axis=-1)


def _swiglu(h, w_in, w_out):
    g, u = jnp.split(h @ w_in, 2, axis=-1)
    return (jax.nn.silu(g) * u) @ w_out


def _lin_rec(e1, e2):
    a1, b1 = e1
    a2, b2 = e2
    return a1 * a2, a2 * b1 + b2


def _s5_discretize(lam_re, lam_im, log_step, b_re, b_im):
    f32 = jnp.float32
    lam = lax.complex(lam_re.astype(f32), lam_im.astype(f32))
    lam_bar = jnp.exp(lam * jnp.exp(log_step.astype(f32)))
    b = lax.complex(b_re.astype(f32), b_im.astype(f32))
    b_bar = ((lam_bar - 1.0) / lam)[..., None] * b
    return lam_bar, b_bar


def _s5_scan(bu, lam_bar, h0, reverse):
    first, last = (-1, 0) if reverse else (0, -1)
    bu = bu.at[:, first].add(lam_bar * h0)
    a = jnp.broadcast_to(lam_bar, bu.shape)
    _, h = lax.associative_scan(_lin_rec, (a, bu), reverse=reverse, axis=1)
    return h, h[:, last]


def s5_mixer(u_c, u_x, lam_re, lam_im, log_step, b_re, b_im, c_re, c_im, d_skip, w_glu, b_glu, need_ctx):
    f32 = jnp.float32
    uc = u_c.astype(f32).reshape(u_c.shape[:2] + (S5_G, S5_CH))
    ux = u_x.astype(f32).reshape(u_x.shape[:2] + (S5_G, S5_CH))
    d = d_skip.astype(f32).reshape(S5_G, S5_CH)
    y_c = d * uc
    y_x = d * ux
    h0 = jnp.zeros((ux.shape[0], S5_G, S5_P), jnp.complex64)
    for di, reverse in enumerate((False, True)):
        lam_bar, b_bar = _s5_discretize(lam_re[di], lam_im[di], log_step[di], b_re[di], b_im[di])
        c_mat = lax.complex(c_re[di].astype(f32), c_im[di].astype(f32))
        h_c, h_fin = _s5_scan(jnp.einsum('blgc,gpc->blgp', uc, b_bar), lam_bar, h0, reverse)
        h_x, _ = _s5_scan(jnp.einsum('blgc,gpc->blgp', ux, b_bar), lam_bar, h_fin, reverse)
        y_x = y_x + jnp.einsum('blgp,gcp->blgc', h_x, c_mat).real
        if need_ctx:
            y_c = y_c + jnp.einsum('blgp,gcp->blgc', h_c, c_mat).real

    def glu(y, like):
        g = jax.nn.gelu(y.reshape(y.shape[:2] + (S5_W,))).astype(like.dtype)
        return g * jax.nn.sigmoid(g @ w_glu + b_glu)

    out_x = glu(y_x, u_x)
    out_c = glu(y_c, u_c) if need_ctx else None
    return out_c, out_x


def conv_module(pw, dw_w, dw_b, ln_g, ln_b, rows):
    a, b = jnp.split(pw, 2, axis=-1)
    g = a * jax.nn.sigmoid(b)
    bsz, length, ch = g.shape
    seqs = g.reshape(bsz * rows, length // rows, ch)
    y = lax.conv_general_dilated(seqs, dw_w[:, None, :], window_strides=(1,),
                                 padding=[(CONV_K // 2, CONV_K // 2)],
                                 dimension_numbers=('NWC', 'WIO', 'NWC'),
                                 feature_group_count=ch)
    y = y.reshape(bsz, length, ch) + dw_b
    return jax.nn.silu(_layernorm(y, ln_g, ln_b))


def _chunks(t):
    bsz, length = t.shape[:2]
    t = t.reshape((bsz, length // ML_CHUNK, ML_CHUNK) + t.shape[2:])
    return jnp.swapaxes(jnp.moveaxis(t, 1, 0), 2, 3)


def _unchunk(t):
    t = jnp.moveaxis(jnp.swapaxes(t, 2, 3), 0, 1)
    return t.reshape((t.shape[0], -1) + t.shape[3:])


def _mlstm_chunkwise(q, k, v, log_i, log_f, state):
    tril = jnp.tril(jnp.ones((ML_CHUNK, ML_CHUNK), bool))

    def step(carry, xs):
        cm, n, m = carry
        qc, kc, vc, li, lf = xs
        b = jnp.cumsum(lf, axis=-1)
        d_log = jnp.where(tril, b[..., :, None] - b[..., None, :] + li[..., None, :], -jnp.inf)
        inter = b + m[..., None]
        m_row = jnp.maximum(inter, jnp.max(d_log, axis=-1))
        s = jnp.einsum('bhjd,bhsd->bhjs', qc, kc) * jnp.exp(d_log - m_row[..., None])
        w_inter = jnp.exp(inter - m_row)
        num = jnp.einsum('bhjs,bhsv->bhjv', s, vc) + w_inter[..., None] * jnp.einsum('bhjd,bhdv->bhjv', qc, cm)
        den = jnp.sum(s, axis=-1) + w_inter * jnp.einsum('bhjd,bhd->bhj', qc, n)
        h = num / jnp.maximum(jnp.abs(den), jnp.exp(-m_row))[..., None]
        g = b[..., -1:] - b + li
        m_new = jnp.maximum(b[..., -1] + m, jnp.max(g, axis=-1))
        kw = kc * jnp.exp(g - m_new[..., None])[..., None]
        decay = jnp.exp(b[..., -1] + m - m_new)
        cm = decay[..., None, None] * cm + jnp.einsum('bhsd,bhsv->bhdv', kw, vc)
        n = decay[..., None] * n + jnp.sum(kw, axis=-2)
        return (cm, n, m_new), h

    xs = (_chunks(q), _chunks(k), _chunks(v), _chunks(log_i), _chunks(log_f))
    state, h = lax.scan(step, state, xs)
    return _unchunk(h), state


def _mlstm_dir(q, k, v, li, lf, state, reverse):
    if reverse:
        q, k, v, li, lf = (jnp.flip(t, axis=1) for t in (q, k, v, li, lf))
    h, state = _mlstm_chunkwise(q, k, v, li, lf, state)
    return (jnp.flip(h, axis=1) if reverse else h), state


def _mlstm_inputs(z):
    bsz, length = z.shape[:2]
    f32 = jnp.float32
    q = z[..., OFF_Q:OFF_K].astype(f32).reshape(bsz, length, ML_H, ML_DH)
    k = z[..., OFF_K:OFF_V].astype(f32).reshape(bsz, length, ML_H, ML_DH) * (ML_DH ** -0.5)
    v = z[..., OFF_V:OFF_O].astype(f32).reshape(bsz, length, ML_H, ML_DH)
    gates = z[..., OFF_G:IN_W].astype(f32).reshape(bsz, length, 4, ML_H)
    log_i = (gates[:, :, 0], gates[:, :, 2])
    log_f = (jax.nn.log_sigmoid(gates[:, :, 1]), jax.nn.log_sigmoid(gates[:, :, 3]))
    return q, k, v, log_i, log_f


def _mlstm_readout(h, z, norm_g):
    bsz, length = h.shape[:2]
    hc = h - jnp.mean(h, axis=-1, keepdims=True)
    var = jnp.mean(hc * hc, axis=-1, keepdims=True)
    h = (hc * lax.rsqrt(var + LN_EPS)).reshape(bsz, length, ML_W) * norm_g.astype(jnp.float32)
    o = jax.nn.sigmoid(z[..., OFF_O:OFF_G].astype(jnp.float32))
    return (o * h).astype(z.dtype)


def mlstm_mixer(zc, zx, norm_g, need_ctx):
    qc, kc, vc, lic, lfc = _mlstm_inputs(zc)
    qx, kx, vx, lix, lfx = _mlstm_inputs(zx)
    bsz = zx.shape[0]
    f32 = jnp.float32
    zero = (jnp.zeros((bsz, ML_H, ML_DH, ML_DH), f32), jnp.zeros((bsz, ML_H, ML_DH), f32),
            jnp.zeros((bsz, ML_H), f32))
    h_c_sum = 0.0
    h_x_sum = 0.0
    for di, reverse in enumerate((False, True)):
        h_c, st = _mlstm_dir(qc, kc, vc, lic[di], lfc[di], zero, reverse)
        h_x, _ = _mlstm_dir(qx, kx, vx, lix[di], lfx[di], st, reverse)
        h_c_sum = h_c_sum + h_c
        h_x_sum = h_x_sum + h_x
    out_x = _mlstm_readout(h_x_sum, zx, norm_g)
    out_c = _mlstm_readout(h_c_sum, zc, norm_g) if need_ctx else None
    return out_c, out_x


def setup_inputs(seed: int = 0) -> dict:
    key = jax.random.key(seed)
    ks = jax.random.split(key, 32)
    f32 = jnp.float32
    L = DEPTH

    def nrm(k, shape, s):
        return s * jax.random.normal(k, shape, f32)

    f_bias = jnp.linspace(3.0, 6.0, ML_H, dtype=f32)
    b_in = nrm(ks[8], (L, IN_W), 0.02)
    b_in = b_in.at[:, OFF_G + ML_H:OFF_G + 2 * ML_H].add(f_bias).at[:, OFF_G + 3 * ML_H:].add(f_bias)
    return {
        'x': nrm(ks[0], (BATCH, SEQ, D_MODEL), 1.0),
        'c': nrm(ks[1], (BATCH, D_MODEL), 1.0),
        'ctx': nrm(ks[2], (BATCH, CTX_LEN, D_MODEL), 1.0),
        'c_ctx': nrm(ks[3], (D_MODEL,), 1.0),
        'w_mod': nrm(ks[4], (L, D_MODEL, 6 * D_MODEL), 0.5 * D_MODEL ** -0.5),
        'b_mod': nrm(ks[5], (L, 6 * D_MODEL), 0.02),
        'norm1_g': 1.0 + nrm(ks[6], (L, D_MODEL), 0.02),
        'w_in': nrm(ks[7], (L, D_MODEL, IN_W), D_MODEL ** -0.5),
        'b_in': b_in,
        's5_lam_re': -0.5 + nrm(ks[9], (L, 2, S5_G, S5_P), 0.01),
        's5_lam_im': math.pi * jnp.arange(S5_P, dtype=f32) + nrm(ks[10], (L, 2, S5_G, S5_P), 0.01),
        's5_log_step': jax.random.uniform(ks[11], (L, 2, S5_G, S5_P), f32, math.log(1e-3), math.log(1e-1)),
        's5_b_re': nrm(ks[12], (L, 2, S5_G, S5_P, S5_CH), (2 * S5_CH) ** -0.5),
        's5_b_im': nrm(ks[13], (L, 2, S5_G, S5_P, S5_CH), (2 * S5_CH) ** -0.5),
        's5_c_re': nrm(ks[14], (L, 2, S5_G, S5_CH, S5_P), S5_P ** -0.5),
        's5_c_im': nrm(ks[15], (L, 2, S5_G, S5_CH, S5_P), S5_P ** -0.5),
        's5_d': nrm(ks[16], (L, S5_W), 1.0),
        's5_w_glu': nrm(ks[17], (L, S5_W, S5_W), S5_W ** -0.5),
        's5_b_glu': nrm(ks[18], (L, S5_W), 0.02),
        'conv_dw_w': nrm(ks[19], (L, CONV_K, CONV_W), CONV_K ** -0.5),
        'conv_dw_b': nrm(ks[20], (L, CONV_W), 0.02),
        'conv_ln_g': 1.0 + nrm(ks[21], (L, CONV_W), 0.02),
        'conv_ln_b': nrm(ks[22], (L, CONV_W), 0.02),
        'ml_norm_g': 1.0 + nrm(ks[23], (L, ML_W), 0.02),
        'w_out': nrm(ks[24], (L, MIX_W, D_MODEL), MIX_W ** -0.5),
        'norm2_g': 1.0 + nrm(ks[25], (L, D_MODEL), 0.02),
        'w_ffn_in': nrm(ks[26], (L, D_MODEL, 2 * D_FF), D_MODEL ** -0.5),
        'w_ffn_out': nrm(ks[27], (L, D_FF, D_MODEL), D_FF ** -0.5),
        'norm_f_g': 1.0 + nrm(ks[28], (D_MODEL,), 0.02),
    }


def reference(x, c, ctx, c_ctx, w_mod, b_mod, norm1_g, w_in, b_in, s5_lam_re, s5_lam_im,
              s5_log_step, s5_b_re, s5_b_im, s5_c_re, s5_c_im, s5_d, s5_w_glu, s5_b_glu,
              conv_dw_w, conv_dw_b, conv_ln_g, conv_ln_b, ml_norm_g, w_out, norm2_g,
              w_ffn_in, w_ffn_out, norm_f_g):
    rows = x.shape[1] // GRID_W
    for l in range(DEPTH):
        need_ctx = l < DEPTH - 1
        sh1_x, sc1_x, g1_x, sh2_x, sc2_x, g2_x = _adaln(c, w_mod[l], b_mod[l])
        sh1_c, sc1_c, g1_c, sh2_c, sc2_c, g2_c = _adaln(c_ctx, w_mod[l], b_mod[l])

        hx = _rmsnorm(x, norm1_g[l]) * (1.0 + sc1_x) + sh1_x
        hc = _rmsnorm(ctx, norm1_g[l]) * (1.0 + sc1_c) + sh1_c
        zx = hx @ w_in[l] + b_in[l]
        zc = hc @ w_in[l] + b_in[l]
        s5_c, s5_x = s5_mixer(zc[..., OFF_S5:OFF_CONV], zx[..., OFF_S5:OFF_CONV],
                              s5_lam_re[l], s5_lam_im[l], s5_log_step[l], s5_b_re[l], s5_b_im[l],
                              s5_c_re[l], s5_c_im[l], s5_d[l], s5_w_glu[l], s5_b_glu[l], need_ctx)
        ml_c, ml_x = mlstm_mixer(zc, zx, ml_norm_g[l], need_ctx)
        cv_x = conv_module(zx[..., OFF_CONV:OFF_Q], conv_dw_w[l], conv_dw_b[l],
                           conv_ln_g[l], conv_ln_b[l], rows)
        x = x + g1_x * (jnp.concatenate([s5_x, cv_x, ml_x], axis=-1) @ w_out[l])

        hx2 = _rmsnorm(x, norm2_g[l]) * (1.0 + sc2_x) + sh2_x
        x = x + g2_x * _swiglu(hx2, w_ffn_in[l], w_ffn_out[l])

        if need_ctx:
            cv_c = conv_module(zc[..., OFF_CONV:OFF_Q], conv_dw_w[l], conv_dw_b[l],
                               conv_ln_g[l], conv_ln_b[l], 1)
            ctx = ctx + g1_c * (jnp.concatenate([s5_c, cv_c, ml_c], axis=-1) @ w_out[l])
            hc2 = _rmsnorm(ctx, norm2_g[l]) * (1.0 + sc2_c) + sh2_c
            ctx = ctx + g2_c * _swiglu(hc2, w_ffn_in[l], w_ffn_out[l])
    return _rmsnorm(x, norm_f_g)
```

```python
import os
import numpy as np
import concourse.bass as bass
import concourse.mybir as mybir
from concourse.bass_utils import run_bass_kernel_spmd

F32 = mybir.dt.float32
BF16 = mybir.dt.bfloat16
AF = mybir.ActivationFunctionType
ALU = mybir.AluOpType
AX = mybir.AxisListType

D = 2048
KT = 16
DEPTH = 4
SEQ = 8192
NCORE = 8
XTOK = 1024
CTX = 256
T = CTX + XTOK
NTILE = T // 128
IN_W = 5648
D_FF = 5632
EPS = 1e-6
LN_EPS = 1e-5
TB = [(0, 256, 1), (256, 512, 0), (768, 512, 0)]

ARENA_LO = 16640
ARENA_HI = 229376


def _prod(xs):
    r = 1
    for v in xs:
        r *= int(v)
    return r


class Buf:
    __slots__ = ("name", "w", "r", "so")

    def __init__(self, name=""):
        self.name = name
        self.w = None
        self.r = {}
        self.so = None


class SemObj:
    __slots__ = ("sem", "tot", "owner")

    def __init__(self, sem):
        self.sem = sem
        self.tot = 0
        self.owner = None


class Tl:
    def __init__(self, t, b):
        self.t = t
        self.b = b

    def __getitem__(self, k):
        return self.t[k]


def _b(x):
    return x.b if isinstance(x, Tl) else x


class Prog:
    ENG = ("pe", "act", "dve", "pool", "sp")
    CE = ("pe", "act", "dve", "pool")

    def __init__(self, nc):
        self.nc = nc
        self.q = {e: [] for e in self.ENG}
        self.sem = {e: nc.alloc_semaphore("s_" + e) for e in self.CE}
        self.cnt = {e: 0 for e in self.CE}
        self.seen = {e: {} for e in self.ENG}
        self.pool = [SemObj(nc.alloc_semaphore("d%d" % i)) for i in range(80)]
        self.pool_i = 0
        self.top = ARENA_LO
        self.nalloc = 0
        self.regions = []
        self.psum = [Tl(nc.alloc_psum_tensor("psb%d" % i, [128, 512], F32), Buf("ps%d" % i)) for i in range(8)]
        self.psi = 0

    def tile(self, shape, dtype, name="t"):
        nbytes = _prod(shape[1:]) * mybir.dt.size(dtype)
        nbytes = (nbytes + 63) // 64 * 64
        off = self.top
        self.top += nbytes
        assert self.top <= ARENA_HI, "SBUF arena overflow %d" % self.top
        self.nalloc += 1
        nm = "%s_%d" % (name, self.nalloc)
        nb = Buf(nm)
        lo, hi = off, off + nbytes
        keep = []
        for (a, b_, ob) in self.regions:
            if a < hi and lo < b_:
                if ob.w is not None:
                    nb.r[("inh", len(nb.r))] = ob.w
                for tok in ob.r.values():
                    nb.r[("inh", len(nb.r))] = tok
                if not (lo <= a and b_ <= hi):
                    keep.append((a, b_, ob))
            else:
                keep.append((a, b_, ob))
        keep.append((lo, hi, nb))
        self.regions = keep
        return Tl(self.nc.alloc_sbuf_tensor_at(nm, list(shape), dtype, offset=off), nb)

    def mark(self):
        return self.top

    def release(self, m):
        self.top = m

    def dram(self, name, shape, dtype, kind="Internal"):
        return self.nc.dram_tensor(name, list(shape), dtype, kind=kind).ap()

    def ps(self):
        p = self.psum[self.psi % 8]
        self.psi += 1
        return p

    def _wait(self, st, key, sem, val):
        if self.seen[st].get(key, 0) >= val:
            return
        self.seen[st][key] = val
        self.q[st].append(lambda h, sem=sem, val=val: h.wait_ge(sem, val))

    def _deps(self, st, R, W):
        toks = []
        for b in R:
            if b.w is not None:
                toks.append(b.w)
        for b in W:
            if b.w is not None:
                toks.append(b.w)
            toks.extend(b.r.values())
        for kind, obj, val in toks:
            if kind == "e":
                if obj == st and st == "pe":
                    continue
                self._wait(st, obj, self.sem[obj], val)
            else:
                self._wait(st, id(obj), obj.sem, obj.tot)

    def _post(self, tok, key, R, W):
        for b in W:
            b.w = tok
            b.r = {}
        for b in R:
            if b not in W:
                b.r[key] = tok

    def op(self, eng, fn, R=(), W=()):
        R = [_b(x) for x in R]
        W = [_b(x) for x in W]
        self._deps(eng, R, W)
        self.cnt[eng] += 1
        sem = self.sem[eng]
        self.q[eng].append(lambda h, fn=fn, sem=sem: fn(h).then_inc(sem, 1))
        self._post(("e", eng, self.cnt[eng]), eng, R, W)

    def dma(self, q, out, in_, R=(), W=(), owner=None, slow=False):
        R = [_b(x) for x in R]
        W = [_b(x) for x in W]
        owner = _b(owner)
        self._deps(q, R, W)
        so = owner.so
        if so is None or so.owner is not owner:
            so = self.pool[self.pool_i % len(self.pool)]
            self.pool_i += 1
            if so.tot > 0:
                self._wait(q, id(so), so.sem, so.tot)
            so.owner = owner
            owner.so = so
        so.tot += 16
        sem = so.sem
        if slow:
            self.q[q].append(lambda h, out=out, in_=in_, sem=sem: h.dma_start(out=out, in_=in_, allow_slow_non_contiguous=True).then_inc(sem, 16))
        else:
            self.q[q].append(lambda h, out=out, in_=in_, sem=sem: h.dma_start(out=out, in_=in_).then_inc(sem, 16))
        self._post(("d", so, so.tot), ("d", id(so)), R, W)

    def barrier(self):
        for st in self.ENG:
            for e in self.CE:
                if self.cnt[e] > 0 and not (e == st and st == "pe"):
                    self._wait(st, e, self.sem[e], self.cnt[e])
            for so in self.pool:
                if so.tot > 0:
                    self._wait(st, id(so), so.sem, so.tot)

    def mm(self, out, lhsT, rhs, start, stop, R, W):
        self.op("pe", lambda h: h.matmul(out, lhsT=lhsT, rhs=rhs, start=start, stop=stop), R, W)

    def tr(self, out, in_, ident, R, W):
        self.op("pe", lambda h: h.transpose(out, in_, ident), R, W)

    def act(self, out, in_, func, R, W, bias=None, scale=None):
        kw = {}
        if bias is not None:
            kw["bias"] = bias
        if scale is not None:
            kw["scale"] = scale
        self.op("act", lambda h: h.activation(out=out, in_=in_, func=func, **kw), R, W)

    def tt(self, eng, out, in0, in1, op, R, W):
        self.op(eng, lambda h: h.tensor_tensor(out=out, in0=in0, in1=in1, op=op), R, W)

    def ts(self, eng, out, in0, s1, s2, op0, op1, R, W):
        if s2 is None:
            self.op(eng, lambda h: h.tensor_scalar(out=out, in0=in0, scalar1=s1, scalar2=None, op0=op0), R, W)
        else:
            self.op(eng, lambda h: h.tensor_scalar(out=out, in0=in0, scalar1=s1, scalar2=s2, op0=op0, op1=op1), R, W)

    def stt(self, out, in0, scalar, in1, op0, op1, R, W):
        self.op("dve", lambda h: h.scalar_tensor_tensor(out=out, in0=in0, scalar=scalar, in1=in1, op0=op0, op1=op1), R, W)

    def copy(self, eng, out, in_, R, W):
        if eng == "act":
            self.op("act", lambda h: h.copy(out=out, in_=in_), R, W)
        else:
            self.op(eng, lambda h: h.tensor_copy(out=out, in_=in_), R, W)

    def memset(self, eng, ap, val, W):
        self.op(eng, lambda h: h.memset(ap, val), (), W)

    def recip(self, out, in_, R, W):
        self.op("dve", lambda h: h.reciprocal(out=out, in_=in_), R, W)

    def scan(self, out, d0, d1, init, op0, op1, R, W):
        self.op("dve", lambda h: h.tensor_tensor_scan(out=out, data0=d0, data1=d1, initial=init, op0=op0, op1=op1), R, W)

    def emit(self):
        nc = self.nc
        self.barrier()
        q = self.q
        with nc.Block() as block:
            @block.tensor
            def _(h):
                for f in q["pe"]:
                    f(h)

            @block.scalar
            def _(h):
                for f in q["act"]:
                    f(h)

            @block.vector
            def _(h):
                for f in q["dve"]:
                    f(h)

            @block.gpsimd
            def _(h):
                for f in q["pool"]:
                    f(h)

            @block.sync
            def _(h):
                for f in q["sp"]:
                    f(h)


class K:
    def __init__(self, nlayers=DEPTH, dbg=(), ncores=NCORE):
        self.ncores = ncores
        self.nlayers = nlayers
        self.dbg = set(dbg)
        nc = bass.Bass("TRN2", target_bir_lowering=False)
        self.nc = nc
        self.P = Prog(nc)
        self.decl_io()

    def inp(self, name, shape):
        return self.nc.dram_tensor(name, list(shape), F32, kind="ExternalInput").ap()

    def scratch(self, name, shape, dtype):
        kind = "ExternalOutput" if name in self.dbg else "Internal"
        return self.nc.dram_tensor(name, list(shape), dtype, kind=kind).ap()

    def decl_io(self):
        L = DEPTH
        self.xT = self.inp("xT", [D, XTOK])
        self.ctxT = self.inp("ctxT", [D, CTX])
        self.cc = self.inp("cc", [D, 2])
        self.w_mod = self.inp("w_mod", [L, D, 6 * D])
        self.b_mod = self.inp("b_mod", [L, 6 * D])
        self.norm1_g = self.inp("norm1_g", [L, D])
        self.w_in = self.inp("w_in", [L, D, IN_W])
        self.b_in = self.inp("b_in", [L, IN_W])
        self.w_out = self.inp("w_out", [L, D, D])
        self.norm2_g = self.inp("norm2_g", [L, D])
        self.w_ffn_in = self.inp("w_ffn_in", [L, D, 2 * D_FF])
        self.w_ffn_out = self.inp("w_ffn_out", [L, D_FF, D])
        self.norm_f_g = self.inp("norm_f_g", [D])
        self.conv_dw_w = self.inp("conv_dw_w", [L, 31, 512])
        self.conv_dw_b = self.inp("conv_dw_b", [L, 512])
        self.conv_ln_g = self.inp("conv_ln_g", [L, 512])
        self.conv_ln_b = self.inp("conv_ln_b", [L, 512])
        self.ml_norm_g = self.inp("ml_norm_g", [L, 1024])
        self.s5_lam_re = self.inp("s5_lam_re", [L, 2, 32, 64])
        self.s5_lam_im = self.inp("s5_lam_im", [L, 2, 32, 64])
        self.s5_log_step = self.inp("s5_log_step", [L, 2, 32, 64])
        self.s5_b_re = self.inp("s5_b_re", [L, 2, 32, 64, 16])
        self.s5_b_im = self.inp("s5_b_im", [L, 2, 32, 64, 16])
        self.s5_c_re = self.inp("s5_c_re", [L, 2, 32, 16, 64])
        self.s5_c_im = self.inp("s5_c_im", [L, 2, 32, 16, 64])
        self.s5_d = self.inp("s5_d", [L, 512])
        self.s5_w_glu = self.inp("s5_w_glu", [L, 512, 512])
        self.s5_b_glu = self.inp("s5_b_glu", [L, 512])
        self.c16 = self.inp("c16", [16, 24])
        self.c128 = self.inp("c128", [128, 5, 128])
        self.c4 = self.inp("c4", [4, 4, 128])
        self.cflags = self.inp("cflags", [128, 16])
        self.outT = self.nc.dram_tensor("outT", [D, XTOK], F32, kind="ExternalOutput").ap()
        self.XT = self.scratch("XT", [KT, 128, T], F32)
        self.XTb = {(kt, bi): Buf("XT%d_%d" % (kt, bi)) for kt in range(KT) for bi in range(len(TB))}
        self.MIXT = self.scratch("MIXT", [KT, 128, T], BF16)
        self.MIXTb = Buf("MIXT")
        self.ZU = self.scratch("ZU", [4, 128, T], BF16)
        self.ZC = self.scratch("ZC", [8, 128, T], F32)
        self.ZQ = self.scratch("ZQ", [8, 128, T], BF16)
        self.ZK = self.scratch("ZK", [8, 128, T], BF16)
        self.ZG = self.scratch("ZG", [16, T], F32)
        self.KTOK = self.scratch("KTOK", [T, 1024], BF16)
        self.VTOK = self.scratch("VTOK", [T, 1024], BF16)
        self.OTOK = self.scratch("OTOK", [T, 1024], F32)
        self.HH = self.scratch("HH", [32, 128, 2, T], F32)
        self.HHb = Buf("HH")
        self.PLF = 8 * 514 + 8 + 128
        self.XIs = [self.nc.dram_tensor("xchg_in%d" % i, [128, self.PLF], F32) for i in range(DEPTH)]
        self.XOs = [self.nc.dram_tensor("xchg_out%d" % i, [NCORE * 128, self.PLF], F32) for i in range(DEPTH)]
        self.XI, self.XO = self.XIs[0], self.XOs[0]
        self.XIb = Buf("XI")
        self.SCD = self.scratch("SCD", [128, 8, 514], F32)
        self.SCDb = Buf("SCD")
        self.XOb = Buf("XO")
        self.ZTb = Buf("ZT")

    def dump(self, name, tl, shape, dtype=F32):
        if name not in self.dbg:
            return
        d = self.nc.dram_tensor(name, list(shape), dtype, kind="ExternalOutput").ap()
        self.P.dma("sp", d, tl[:], [tl], (), owner=tl)
        self.P.barrier()

    def setup_consts(self):
        P = self.P
        self.ones_bf = P.tile([128, 128], BF16, "ones_bf")
        P.memset("dve", self.ones_bf[:], 1.0, [self.ones_bf])
        self.ones_f = P.tile([128, 128], F32, "ones_f")
        P.memset("dve", self.ones_f[:], 1.0, [self.ones_f])
        self.eps_c = P.tile([128, 2], F32, "eps")
        P.memset("dve", self.eps_c[:], EPS, [self.eps_c])
        self.lneps_c = P.tile([128, 2], F32, "lneps")
        P.memset("dve", self.lneps_c[:], LN_EPS, [self.lneps_c])
        self.one_c = P.tile([128, 2], F32, "one_c")
        P.memset("dve", self.one_c[:], 1.0, [self.one_c])
        ccT = P.tile([128, KT, 2], F32, "ccT")
        P.dma("sp", ccT[:], self.cc.rearrange("(kt p) j -> p kt j", p=128), (), [ccT], owner=ccT, slow=True)
        self.scT = P.tile([128, KT, 2], BF16, "scT")
        P.act(self.scT[:], ccT[:], AF.Silu, [ccT], [self.scT])
        self.wslots = None
        self.wsi = 0
        self.mod = P.tile([128, 6 * KT, 2, 2], F32, "mod")
        self.gm1 = P.tile([128, KT, 2], F32, "gm1")
        self.gm2 = P.tile([128, KT, 2], F32, "gm2")

    def new_wslots(self):
        self.wslots = [self.P.tile([128, KT, 512], BF16, "wslot%d" % i) for i in range(3)]

    def wslot(self):
        s = self.wslots[self.wsi % len(self.wslots)]
        self.wsi += 1
        return s

    def load_w(self, src, r0, nk, c0, nc_):
        s = self.wslot()
        self.P.dma("pool", s[:, 0:nk, 0:nc_],
                   src[r0:r0 + nk * 128, c0:c0 + nc_].rearrange("(kt p) c -> p kt c", p=128),
                   (), [s], owner=s)
        return s

    def stage_mod(self, l):
        P = self.P
        m0 = P.mark()
        self.new_wslots()
        bm = P.tile([128, 6 * KT], F32, "bmodT")
        P.dma("sp", bm[:], self.b_mod[l].rearrange("(j p) -> p j", p=128), (), [bm], owner=bm, slow=True)
        n1 = P.tile([128, KT], F32, "n1g")
        P.dma("sp", n1[:], self.norm1_g[l].rearrange("(j p) -> p j", p=128), (), [n1], owner=n1, slow=True)
        n2 = P.tile([128, KT], F32, "n2g")
        P.dma("sp", n2[:], self.norm2_g[l].rearrange("(j p) -> p j", p=128), (), [n2], owner=n2, slow=True)
        ps = P.ps()
        for cb in range(24):
            w = self.load_w(self.w_mod[l], 0, KT, cb * 512, 512)
            for jj in range(4):
                j = cb * 4 + jj
                for kt in range(KT):
                    P.mm(ps[:, 2 * j:2 * j + 2], w[:, kt, jj * 128:(jj + 1) * 128], self.scT[:, kt, :],
                         kt == 0, kt == KT - 1, [w, self.scT], [ps])
        mod = self.mod
        P.tt("dve", mod[:, :, :, 0], ps[:, 0:192].rearrange("p (j w) -> p j w", w=2),
             bm[:].unsqueeze(2).to_broadcast([128, 6 * KT, 2]), ALU.add, [ps, bm], [mod])
        for (gm, ng, v) in ((self.gm1, n1, 1), (self.gm2, n2, 4)):
            P.ts("dve", gm[:], mod[:, v * KT:(v + 1) * KT, :, 0], 1.0, None, ALU.add, None, [mod], [gm])
            P.tt("dve", gm[:], gm[:], ng[:].unsqueeze(2).to_broadcast([128, KT, 2]), ALU.mult, [gm, ng], [gm])
        P.barrier()
        self.dump("d_mod%d" % l, mod, [128, 6 * KT, 2, 2])
        P.release(m0)

    def stage_load_x(self):
        P = self.P
        m0 = P.mark()
        for kt in range(KT):
            st = P.tile([128, T], F32, "xld")
            P.dma("sp", st[:, 0:CTX], self.ctxT[kt * 128:(kt + 1) * 128, :], (), [st], owner=st)
            P.dma("sp", st[:, CTX:T], self.xT[kt * 128:(kt + 1) * 128, :], (), [st], owner=st)
            bufs = [self.XTb[(kt, bi)] for bi in range(len(TB))]
            P.dma("sp", self.XT[kt], st[:], [st], bufs, owner=st)
            if kt % 4 == 3:
                P.barrier()
                P.release(m0)
        P.barrier()
        P.release(m0)

    def rsqrt(self, out, src, n, scale, eps_t, R):
        P = self.P
        m0 = P.mark()
        v = P.tile([128, n], F32, "rs_v")
        t = P.tile([128, n], F32, "rs_t")
        P.ts("dve", v[:], src, scale, eps_t[:, 0:1], ALU.mult, ALU.add, list(R) + [eps_t], [v])
        P.act(out[:, 0:n], v[:], AF.Sqrt, [v], [out])
        P.recip(out[:, 0:n], out[:, 0:n], [out], [out])
        P.tt("dve", t[:], v[:], out[:, 0:n], ALU.mult, [v, out], [t])
        P.tt("dve", t[:], t[:], out[:, 0:n], ALU.mult, [t, out], [t])
        P.ts("dve", t[:], t[:], -0.5, 1.5, ALU.mult, ALU.add, [t], [t])
        P.tt("dve", out[:, 0:n], out[:, 0:n], t[:], ALU.mult, [out, t], [out])
        P.release(m0)

    def stage_norm(self, hT, gm, shift_v):
        P = self.P
        m0 = P.mark()
        for bi, (t0, n, wh) in enumerate(TB):
            m1 = P.mark()
            xt = P.tile([128, KT, n], F32, "nx")
            for kt in range(KT):
                P.dma("sp" if kt % 2 == 0 else "act", xt[:, kt, :], self.XT[kt, :, t0:t0 + n], [self.XTb[(kt, bi)]], [xt], owner=xt)
            sq = P.tile([128, KT, n], BF16, "nsq")
            P.act(sq[:], xt[:], AF.Square, [xt], [sq])
            ps = P.ps()
            for kt in range(KT):
                P.mm(ps[:, 0:n], self.ones_bf[:], sq[:, kt, :], kt == 0, kt == KT - 1, [sq, self.ones_bf], [ps])
            rstd = P.tile([128, n], F32, "rstd")
            self.rsqrt(rstd, ps[:, 0:n], n, 1.0 / D, self.eps_c, [ps])
            tmp = [P.tile([128, n], F32, "ntmp%d" % i) for i in range(2)]
            for kt in range(KT):
                tp = tmp[kt % 2]
                P.stt(tp[:], xt[:, kt, :], gm[:, kt, wh:wh + 1], rstd[:], ALU.mult, ALU.mult, [xt, gm, rstd], [tp])
                P.act(hT[:, kt, t0:t0 + n], tp[:], AF.Identity, [tp, self.mod], [hT],
                      bias=self.mod[:, shift_v * KT + kt, wh, 0:1])
            P.release(m1)
        P.release(m0)

    def proj_T(self, hT, nk, wsrc, r0, c0, ncols, evac, pre=None):
        P = self.P
        ncb = (ncols + 511) // 512
        for cb in range(ncb):
            cw = min(512, ncols - cb * 512)
            w = self.load_w(wsrc, r0, nk, c0 + cb * 512, cw)
            for jj in range((cw + 127) // 128):
                mw = min(128, cw - jj * 128)
                for bi, (t0, n, wh) in enumerate(TB):
                    if pre is not None:
                        pre(cb * 4 + jj, bi)
                    ps = P.ps()
                    for kt in range(nk):
                        P.mm(ps[0:mw, 0:n], w[:, kt, jj * 128:jj * 128 + mw], hT[:, kt, t0:t0 + n],
                             kt == 0, kt == nk - 1, [w, hT], [ps])
                    evac(cb * 4 + jj, bi, ps)

    def stage_proj_in(self, l, hT):
        P = self.P
        m0 = P.mark()
        binT = P.tile([128, 44], F32, "binT")
        P.dma("sp", binT[:], self.b_in[l, 0:5632].rearrange("(j p) -> p j", p=128), (), [binT], owner=binT, slow=True)
        bing = P.tile([16, 1], F32, "bing")
        P.dma("sp", bing[:], self.b_in[l, 5632:5648].rearrange("(p o) -> p o", o=1), (), [bing], owner=bing, slow=True)
        stg = {}

        def staging(key, dtype):
            if key not in stg:
                stg[key] = {"i": 0, "t": [P.tile([128, 512], dtype, "stg") for _ in range(3)]}
            lst = stg[key]
            s = lst["t"][lst["i"] % 3]
            lst["i"] += 1
            return s

        def evac(j, bi, ps):
            t0, n, wh = TB[bi]
            if j < 4:
                dst, idx, dt, sc = self.ZU, j, BF16, 1.0
            elif j < 12:
                dst, idx, dt, sc = self.ZC, j - 4, F32, 1.0
            elif j < 20:
                dst, idx, dt, sc = self.ZQ, j - 12, BF16, 1.0
            else:
                dst, idx, dt, sc = self.ZK, j - 20, BF16, 1.0 / 16.0
            s = staging(dt, dt)
            if sc == 1.0:
                P.ts("dve", s[:, 0:n], ps[:, 0:n], binT[:, j:j + 1], None, ALU.add, None, [ps, binT], [s])
            else:
                P.ts("dve", s[:, 0:n], ps[:, 0:n], binT[:, j:j + 1], sc, ALU.add, ALU.mult, [ps, binT], [s])
            P.dma("sp", dst[idx, :, t0:t0 + n], s[:, 0:n], [s], [self.ZTb], owner=s)

        self.proj_T(hT, KT, self.w_in[l], 0, 0, 28 * 128, evac)

        def evac_g(j, bi, ps):
            t0, n, wh = TB[bi]
            s = staging(F32, F32)
            P.ts("dve", s[0:16, 0:n], ps[0:16, 0:n], bing[:, 0:1], None, ALU.add, None, [ps, bing], [s])
            P.dma("sp", self.ZG[:, t0:t0 + n], s[0:16, 0:n], [s], [self.ZTb], owner=s)

        self.proj_T(hT, KT, self.w_in[l], 0, 5632, 16, evac_g)
        P.barrier()
        P.release(m0)


    def stage_proj_tok(self, l, hT):
        P = self.P
        m0 = P.mark()
        bb = P.tile([128, 3072], F32, "bkvo")
        P.dma("sp", bb[:], self.b_in[l, 2560:5632].partition_broadcast(128), (), [bb], owner=bb, slow=True)
        stg_b = [P.tile([128, 512], BF16, "stb%d" % i) for i in range(3)]
        stg_f = [P.tile([128, 512], F32, "stf%d" % i) for i in range(3)]
        cnt = 0
        for cb in range(6):
            w = self.load_w(self.w_in[l], 0, KT, 2560 + cb * 512, 512)
            for i in range(NTILE):
                ps = P.ps()
                for kt in range(KT):
                    P.mm(ps[:, 0:512], hT[:, kt, i * 128:(i + 1) * 128], w[:, kt, 0:512], kt == 0, kt == KT - 1, [w, hT], [ps])
                bsl = bb[:, cb * 512:(cb + 1) * 512]
                kind = cb // 2
                c0 = (cb % 2) * 512
                cnt += 1
                if kind == 0:
                    sb = stg_b[cnt % 3]
                    sf = stg_f[cnt % 3]
                    P.tt("dve", sf[:], ps[:, 0:512], bsl, ALU.add, [ps, bb], [sf])
                    P.ts("dve", sb[:], sf[:], 1.0 / 16.0, None, ALU.mult, None, [sf], [sb])
                    P.dma("sp", self.KTOK[i * 128:(i + 1) * 128, c0:c0 + 512], sb[:], [sb], [self.ZTb], owner=sb)
                elif kind == 1:
                    sb = stg_b[cnt % 3]
                    P.tt("dve", sb[:], ps[:, 0:512], bsl, ALU.add, [ps, bb], [sb])
                    P.dma("sp", self.VTOK[i * 128:(i + 1) * 128, c0:c0 + 512], sb[:], [sb], [self.ZTb], owner=sb)
                else:
                    sf = stg_f[cnt % 3]
                    P.tt("dve", sf[:], ps[:, 0:512], bsl, ALU.add, [ps, bb], [sf])
                    P.act(sf[:], sf[:], AF.Sigmoid, [sf], [sf])
                    P.dma("sp", self.OTOK[i * 128:(i + 1) * 128, c0:c0 + 512], sf[:], [sf], [self.ZTb], owner=sf)
        P.barrier()
        P.release(m0)

    def stage_conv(self, l):
        P = self.P
        m0 = P.mark()
        dw = P.tile([128, 4, 32], F32, "dw")
        for ct in range(4):
            P.dma("sp", dw[:, ct, 0:31], self.conv_dw_w[l][:, ct * 128:(ct + 1) * 128].rearrange("k p -> p k"), (), [dw], owner=dw, slow=True)
        prm = P.tile([128, 3, 4, 2], F32, "cprm")
        for i, src in enumerate((self.conv_dw_b, self.conv_ln_g, self.conv_ln_b)):
            P.dma("sp", prm[:, i, :, 0], src[l].rearrange("(ct p) -> p ct", p=128), (), [prm], owner=prm, slow=True)
        Y = P.tile([128, 4, T], F32, "convY")
        for ct in range(4):
            m1 = P.mark()
            a = P.tile([128, T], F32, "ca")
            b = P.tile([128, T], F32, "cb")
            P.dma("sp", a[:], self.ZC[ct], [self.ZTb], [a], owner=a)
            P.dma("act", b[:], self.ZC[4 + ct], [self.ZTb], [b], owner=b)
            P.act(b[:], b[:], AF.Sigmoid, [b], [b])
            P.tt("dve", a[:], a[:], b[:], ALU.mult, [a, b], [a])
            y = Y[:, ct, :]
            P.ts("dve", y, a[:], dw[:, ct, 15:16], prm[:, 0, ct, 0:1], ALU.mult, ALU.add, [a, dw, prm], [Y])
            yx = Y[:, ct, CTX:T].rearrange("p (r w) -> p r w", w=64)
            gx = a[:, CTX:T].rearrange("p (r w) -> p r w", w=64)
            for k in range(31):
                o = k - 15
                if o == 0:
                    continue
                d0, d1 = max(0, -o), CTX - max(0, o)
                P.stt(Y[:, ct, d0:d1], a[:, d0 + o:d1 + o], dw[:, ct, k:k + 1], Y[:, ct, d0:d1], ALU.mult, ALU.add, [a, dw, Y], [Y])
                d0, d1 = max(0, -o), 64 - max(0, o)
                P.stt(yx[:, :, d0:d1], gx[:, :, d0 + o:d1 + o], dw[:, ct, k:k + 1], yx[:, :, d0:d1], ALU.mult, ALU.add, [a, dw, Y], [Y])
            P.release(m1)
        Ysq = P.tile([128, 4, T], F32, "convYsq")
        P.act(Ysq[:], Y[:], AF.Square, [Y], [Ysq])
        stg = [P.tile([128, 512], BF16, "cstg%d" % i) for i in range(3)]
        cnt = 0
        for bi, (t0, n, wh) in enumerate(TB):
            m1 = P.mark()
            ps_s = P.ps()
            ps_q = P.ps()
            for ct in range(4):
                P.mm(ps_s[:, 0:n], self.ones_f[:], Y[:, ct, t0:t0 + n], ct == 0, ct == 3, [Y, self.ones_f], [ps_s])
            for ct in range(4):
                P.mm(ps_q[:, 0:n], self.ones_f[:], Ysq[:, ct, t0:t0 + n], ct == 0, ct == 3, [Ysq, self.ones_f], [ps_q])
            mean = P.tile([128, n], F32, "cmean")
            P.ts("dve", mean[:], ps_s[:, 0:n], 1.0 / 512.0, None, ALU.mult, None, [ps_s], [mean])
            var = P.tile([128, n], F32, "cvar")
            P.tt("dve", var[:], mean[:], mean[:], ALU.mult, [mean], [var])
            P.stt(var[:], ps_q[:, 0:n], 1.0 / 512.0, var[:], ALU.mult, ALU.subtract, [ps_q, var], [var])
            rstd = P.tile([128, n], F32, "crstd")
            self.rsqrt(rstd, var[:], n, 1.0, self.lneps_c, [var])
            tmp = [P.tile([128, n], F32, "ctmp%d" % i) for i in range(2)]
            for ct in range(4):
                tp = tmp[ct % 2]
                P.tt("dve", tp[:], Y[:, ct, t0:t0 + n], mean[:], ALU.subtract, [Y, mean], [tp])
                P.tt("dve", tp[:], tp[:], rstd[:], ALU.mult, [tp, rstd], [tp])
                sb = stg[cnt % 3]
                cnt += 1
                P.act(sb[:, 0:n], tp[:], AF.Silu, [tp, prm], [sb], bias=prm[:, 2, ct, 0:1], scale=prm[:, 1, ct, 0:1])
                P.dma("sp", self.MIXT[4 + ct, :, t0:t0 + n], sb[:, 0:n], [sb], [self.MIXTb], owner=sb)
            P.release(m1)
        P.barrier()
        P.release(m0)


    def s5_setup(self, l, need_B=True):
        P = self.P
        S = {}
        TWO_PI = 2.0 * np.pi
        c128 = P.tile([128, 5, 128], F32, "c128s")
        P.dma("sp", c128[:], self.c128, (), [c128], owner=c128)
        identf = c128[:, 2, :]
        cosT = P.tile([128, 32, 128], F32, "cosT")
        sinT = P.tile([128, 32, 128], F32, "sinT")
        LB = P.tile([128, 2, 2, 16, 128], BF16, "LB") if need_B else None
        CEX = P.tile([128, 2, 2, 16, 128], BF16, "CEX")
        prm = P.tile([128, 8, 32], F32, "s5prm")
        dsk = P.tile([128, 4, 2], F32, "dskip")
        P.dma("sp", dsk[:, :, 0], self.s5_d[l].rearrange("(j p) -> p j", p=128), (), [dsk], owner=dsk, slow=True)
        S.update(cosT=cosT, sinT=sinT, LB=LB, CEX=CEX, prm=prm, dsk=dsk)
        m1 = P.mark()
        lr = P.tile([128, 32], F32, "lr")
        li = P.tile([128, 32], F32, "li")
        ls = P.tile([128, 32], F32, "ls")
        for d in range(2):
            for (t_, src) in ((lr, self.s5_lam_re), (li, self.s5_lam_im), (ls, self.s5_log_step)):
                P.dma("sp", t_[:, d * 16:(d + 1) * 16], src[l, d].rearrange("(gp two) p -> (two p) gp", two=2), (), [t_], owner=t_, slow=True)
        w = [P.tile([128, 32], F32, "s5w%d" % i) for i in range(10)]
        dl, ar, ai, r, th, cs, sn, t0_, t1_, t2_ = w
        P.act(dl[:], ls[:], AF.Exp, [ls], [dl])
        P.tt("dve", ar[:], lr[:], dl[:], ALU.mult, [lr, dl], [ar])
        P.tt("dve", ai[:], li[:], dl[:], ALU.mult, [li, dl], [ai])
        P.act(prm[:, 0, :], ar[:], AF.Exp, [ar], [prm])
        ki = P.tile([128, 32], mybir.dt.int32, "s5ki")
        P.ts("dve", t0_[:], ai[:], 1.0 / TWO_PI, 0.5, ALU.mult, ALU.add, [ai], [t0_])
        P.copy("dve", ki[:], t0_[:], [t0_], [ki])
        P.copy("dve", t0_[:], ki[:], [ki], [t0_])
        P.stt(th[:], t0_[:], -TWO_PI, ai[:], ALU.mult, ALU.add, [t0_, ai], [th])

        def sincos(dst_s, dst_c, ang):
            for (dst, shift) in ((dst_s, 0.0), (dst_c, np.pi / 2)):
                P.ts("dve", t1_[:], ang, float(shift), None, ALU.add, None, [th], [t1_])
                for _ in range(2):
                    P.ts("dve", t2_[:], t1_[:], -float(np.pi), None, ALU.is_lt, None, [t1_], [t2_])
                    P.stt(t1_[:], t2_[:], float(TWO_PI), t1_[:], ALU.mult, ALU.add, [t2_, t1_], [t1_])
                    P.ts("dve", t2_[:], t1_[:], float(np.pi), None, ALU.is_gt, None, [t1_], [t2_])
                    P.stt(t1_[:], t2_[:], -float(TWO_PI), t1_[:], ALU.mult, ALU.add, [t2_, t1_], [t1_])
                P.ts("dve", t2_[:], t1_[:], 3.1415925, -3.1415925, ALU.min, ALU.max, [t1_], [t2_])
                P.act(dst, t2_[:], AF.Sin, [t2_], [cs, sn, prm])

        sincos(sn[:], cs[:], th[:])
        nr = P.tile([128, 32], F32, "s5nr")
        ni = P.tile([128, 32], F32, "s5ni")
        P.tt("dve", nr[:], prm[:, 0, :], cs[:], ALU.mult, [prm, cs], [nr])
        P.ts("dve", nr[:], nr[:], -1.0, None, ALU.add, None, [nr], [nr])
        P.tt("dve", ni[:], prm[:, 0, :], sn[:], ALU.mult, [prm, sn], [ni])
        inv = P.tile([128, 32], F32, "s5inv")
        P.tt("dve", inv[:], lr[:], lr[:], ALU.mult, [lr], [inv])
        P.tt("dve", t0_[:], li[:], li[:], ALU.mult, [li], [t0_])
        P.tt("dve", inv[:], inv[:], t0_[:], ALU.add, [inv, t0_], [inv])
        P.recip(inv[:], inv[:], [inv], [inv])
        kr = P.tile([128, 32], F32, "s5kr")
        kim = P.tile([128, 32], F32, "s5kim")
        P.tt("dve", kr[:], nr[:], lr[:], ALU.mult, [nr, lr], [kr])
        P.tt("dve", t0_[:], ni[:], li[:], ALU.mult, [ni, li], [t0_])
        P.tt("dve", kr[:], kr[:], t0_[:], ALU.add, [kr, t0_], [kr])
        P.tt("dve", kr[:], kr[:], inv[:], ALU.mult, [kr, inv], [kr])
        P.tt("dve", kim[:], ni[:], lr[:], ALU.mult, [ni, lr], [kim])
        P.tt("dve", t0_[:], nr[:], li[:], ALU.mult, [nr, li], [t0_])
        P.tt("dve", kim[:], kim[:], t0_[:], ALU.subtract, [kim, t0_], [kim])
        P.tt("dve", kim[:], kim[:], inv[:], ALU.mult, [kim, inv], [kim])
        if need_B:
            Br = P.tile([128, 32, 16], F32, "s5Br")
            Bi = P.tile([128, 32, 16], F32, "s5Bi")
            for d in range(2):
                P.dma("sp", Br[:, d * 16:(d + 1) * 16, :], self.s5_b_re[l, d].rearrange("(gp two) p c -> (two p) gp c", two=2), (), [Br], owner=Br, slow=True)
                P.dma("act", Bi[:, d * 16:(d + 1) * 16, :], self.s5_b_im[l, d].rearrange("(gp two) p c -> (two p) gp c", two=2), (), [Bi], owner=Bi, slow=True)
            Bbr = P.tile([128, 32, 16], F32, "s5Bbr")
            Bbi = P.tile([128, 32, 16], F32, "s5Bbi")
            tb = P.tile([128, 32, 16], F32, "s5tb")
            krb = kr[:].unsqueeze(2).to_broadcast([128, 32, 16])
            kib = kim[:].unsqueeze(2).to_broadcast([128, 32, 16])
            P.tt("dve", Bbr[:], Br[:], krb, ALU.mult, [Br, kr], [Bbr])
            P.tt("dve", tb[:], Bi[:], kib, ALU.mult, [Bi, kim], [tb])
            P.tt("dve", Bbr[:], Bbr[:], tb[:], ALU.subtract, [Bbr, tb], [Bbr])
            P.tt("dve", Bbi[:], Bi[:], krb, ALU.mult, [Bi, kr], [Bbi])
            P.tt("dve", tb[:], Br[:], kib, ALU.mult, [Br, kim], [tb])
            P.tt("dve", Bbi[:], Bbi[:], tb[:], ALU.add, [Bbi, tb], [Bbi])
            BX = P.tile([128, 2, 2, 16, 128], F32, "s5BX")
            P.memset("dve", BX[:], 0.0, [BX])
            for d in range(2):
                for ri, src in enumerate((Bbr, Bbi)):
                    for two in range(2):
                        ps_ = slice(two * 64, (two + 1) * 64)
                        for q in range(4):
                            dst = BX[ps_, d, ri, :, q * 32 + two * 16:q * 32 + two * 16 + 16].rearrange("p (g q) c -> p g q c", q=4)[:, :, q, :]
                            srcv = src[ps_, d * 16:(d + 1) * 16, :].rearrange("p (g q) c -> p g q c", q=4)[:, :, q, :]
                            P.copy("dve", dst, srcv, [src], [BX])
            for d in range(2):
                for ri in range(2):
                    for gp in range(16):
                        ps = P.ps()
                        P.mm(ps[:, 0:128], BX[:, d, ri, gp, :], identf, True, True, [BX, c128], [ps])
                        P.copy("act", LB[:, d, ri, gp, :], ps[:, 0:128], [ps], [LB])
        Cr = P.tile([128, 32, 16], F32, "s5Cr")
        Ci = P.tile([128, 32, 16], F32, "s5Ci")
        for d in range(2):
            for gp in range(16):
                for two in range(2):
                    P.dma("sp", Cr[two * 64:(two + 1) * 64, d * 16 + gp, :], self.s5_c_re[l, d, 2 * gp + two].rearrange("c p -> p c"), (), [Cr], owner=Cr, slow=True)
                    P.dma("act", Ci[two * 64:(two + 1) * 64, d * 16 + gp, :], self.s5_c_im[l, d, 2 * gp + two].rearrange("c p -> p c"), (), [Ci], owner=Ci, slow=True)
        P.ts("dve", Ci[:], Ci[:], -1.0, None, ALU.mult, None, [Ci], [Ci])
        P.memset("dve", CEX[:], 0.0, [CEX])
        for d in range(2):
            for ri, src in enumerate((Cr, Ci)):
                for two in range(2):
                    ps_ = slice(two * 64, (two + 1) * 64)
                    for q in range(4):
                        dst = CEX[ps_, d, ri, :, q * 32 + two * 16:q * 32 + two * 16 + 16].rearrange("p (g q) c -> p g q c", q=4)[:, :, q, :]
                        srcv = src[ps_, d * 16:(d + 1) * 16, :].rearrange("p (g q) c -> p g q c", q=4)[:, :, q, :]
                        P.copy("dve", dst, srcv, [src], [CEX])
        P.memset("dve", cosT[:, :, 0:1], 1.0, [cosT])
        P.memset("dve", sinT[:, :, 0:1], 0.0, [sinT])
        P.copy("dve", prm[:, 4, :], cs[:], [cs], [prm])
        P.copy("dve", prm[:, 5, :], sn[:], [sn], [prm])
        P.ts("dve", prm[:, 6, :], sn[:], -1.0, None, ALU.mult, None, [sn], [prm])
        tA = P.tile([128, 32, 64], F32, "s5tA")
        tB = P.tile([128, 32, 64], F32, "s5tB")
        for k in range(7):
            m = 1 << k
            cb_ = cs[:].unsqueeze(2).to_broadcast([128, 32, m])
            sb_ = sn[:].unsqueeze(2).to_broadcast([128, 32, m])
            P.tt("dve", tA[:, :, 0:m], cosT[:, :, 0:m], cb_, ALU.mult, [cosT, cs], [tA])
            P.tt("dve", tB[:, :, 0:m], sinT[:, :, 0:m], sb_, ALU.mult, [sinT, sn], [tB])
            P.tt("dve", cosT[:, :, m:2 * m], tA[:, :, 0:m], tB[:, :, 0:m], ALU.subtract, [tA, tB], [cosT])
            P.tt("dve", tA[:, :, 0:m], cosT[:, :, 0:m], sb_, ALU.mult, [cosT, sn], [tA])
            P.tt("dve", tB[:, :, 0:m], sinT[:, :, 0:m], cb_, ALU.mult, [sinT, cs], [tB])
            P.tt("dve", sinT[:, :, m:2 * m], tA[:, :, 0:m], tB[:, :, 0:m], ALU.add, [tA, tB], [sinT])
            P.tt("dve", t0_[:], cs[:], cs[:], ALU.mult, [cs], [t0_])
            P.tt("dve", t1_[:], sn[:], sn[:], ALU.mult, [sn], [t1_])
            P.tt("dve", t2_[:], cs[:], sn[:], ALU.mult, [cs, sn], [t2_])
            P.tt("dve", cs[:], t0_[:], t1_[:], ALU.subtract, [t0_, t1_], [cs])
            P.ts("dve", sn[:], t2_[:], 2.0, None, ALU.mult, None, [t2_], [sn])
        P.copy("dve", prm[:, 1, :], cs[:], [cs], [prm])
        P.copy("dve", prm[:, 2, :], sn[:], [sn], [prm])
        P.ts("dve", prm[:, 3, :], sn[:], -1.0, None, ALU.mult, None, [sn], [prm])
        P.release(m1)
        return S

    def s5_more_setup(self, S):
        P = self.P
        prm = S["prm"]
        RP = P.tile([128, 32, 128], F32, "s5RP")
        ex = P.tile([128, 8, 32], F32, "s5ex")
        S["RP"], S["ex"] = RP, ex
        m1 = P.mark()
        rr = P.tile([128, 32], F32, "s5rr")
        P.copy("dve", rr[:], prm[:, 0, :], [prm], [rr])
        P.copy("dve", RP[:, :, 0], prm[:, 0, :], [prm], [RP])
        for k in range(7):
            m = 1 << k
            P.tt("dve", RP[:, :, m:2 * m], RP[:, :, 0:m], rr[:].unsqueeze(2).to_broadcast([128, 32, m]), ALU.mult, [RP, rr], [RP])
            P.tt("dve", rr[:], rr[:], rr[:], ALU.mult, [rr], [rr])
        P.tt("dve", ex[:, 0, :], rr[:], prm[:, 1, :], ALU.mult, [rr, prm], [ex])
        P.tt("dve", ex[:, 1, :], rr[:], prm[:, 2, :], ALU.mult, [rr, prm], [ex])
        P.ts("dve", ex[:, 2, :], ex[:, 1, :], -1.0, None, ALU.mult, None, [ex], [ex])
        lr_ = P.tile([128, 32], F32, "s5Lr")
        li_ = P.tile([128, 32], F32, "s5Li")
        ta = P.tile([128, 32], F32, "s5Lta")
        tb = P.tile([128, 32], F32, "s5Ltb")
        P.copy("dve", lr_[:], ex[:, 0, :], [ex], [lr_])
        P.copy("dve", li_[:], ex[:, 1, :], [ex], [li_])
        for k in range(3):
            P.tt("dve", ta[:], lr_[:], lr_[:], ALU.mult, [lr_], [ta])
            P.tt("dve", tb[:], li_[:], li_[:], ALU.mult, [li_], [tb])
            P.tt("dve", li_[:], lr_[:], li_[:], ALU.mult, [lr_, li_], [li_])
            P.ts("dve", li_[:], li_[:], 2.0, None, ALU.mult, None, [li_], [li_])
            P.tt("dve", lr_[:], ta[:], tb[:], ALU.subtract, [ta, tb], [lr_])
        P.copy("dve", ex[:, 3, :], lr_[:], [lr_], [ex])
        P.copy("dve", ex[:, 4, :], li_[:], [li_], [ex])
        P.ts("dve", ex[:, 5, :], li_[:], -1.0, None, ALU.mult, None, [li_], [ex])
        P.release(m1)

    def stage_s5A(self, l):
        P = self.P
        m0 = P.mark()
        S = self.s5_setup(l)
        cosT, sinT, LB, prm = S["cosT"], S["sinT"], S["LB"], S["prm"]
        ST = P.tile([128, 32, 4], F32, "S5ST")
        self.S5ST = ST
        uT = P.tile([128, 4, T], BF16, "s5uT")
        for j in range(4):
            P.dma("sp", uT[:, j, :], self.ZU[j], [self.ZTb], [uT], owner=uT)
        ghs = [P.tile([128, 2, T], F32, "s5gh%d" % i) for i in range(2)]
        hhs = [P.tile([128, 2, T], F32, "s5hh%d" % i) for i in range(2)]
        tmps = [P.tile([128, 512], F32, "s5tm%d" % i) for i in range(4)]
        ini = [P.tile([128, 4], F32, "s5ini%d" % i) for i in range(2)]
        inic = 0
        it4 = P.tile([128, 4], F32, "s5it4")

        def rv(ap3, d):
            return ap3 if d == 0 else ap3[:, :, ::-1]

        cnt = 0
        for gp in range(16):
            G4 = gp // 4
            for d in range(2):
                col = d * 16 + gp
                gh = ghs[cnt % 2]
                hh_ = hhs[cnt % 2]
                cnt += 1
                ctab = cosT[:, col, :]
                stab = sinT[:, col, :]
                for (t0, n, wh) in TB:
                    nb = n // 128
                    pr = P.ps()
                    pi = P.ps()
                    P.mm(pr[:, 0:n], LB[:, d, 0, gp, :], uT[:, G4, t0:t0 + n], True, True, [LB, uT], [pr])
                    P.mm(pi[:, 0:n], LB[:, d, 1, gp, :], uT[:, G4, t0:t0 + n], True, True, [LB, uT], [pi])
                    cb3 = ctab.unsqueeze(1).to_broadcast([128, nb, 128])
                    sb3 = stab.unsqueeze(1).to_broadcast([128, nb, 128])
                    prv = rv(pr[:, 0:n].rearrange("p (b j) -> p b j", j=128), d)
                    piv = rv(pi[:, 0:n].rearrange("p (b j) -> p b j", j=128), d)
                    tv = [rv(tm[:, 0:n].rearrange("p (b j) -> p b j", j=128), d) for tm in tmps]
                    gre = rv(gh[:, 0, t0:t0 + n].rearrange("p (b j) -> p b j", j=128), d)
                    gim = rv(gh[:, 1, t0:t0 + n].rearrange("p (b j) -> p b j", j=128), d)
                    P.tt("dve", tv[0], prv, cb3, ALU.mult, [pr, cosT], [tmps[0]])
                    P.tt("dve", tv[1], piv, sb3, ALU.mult, [pi, sinT], [tmps[1]])
                    P.tt("dve", tv[2], piv, cb3, ALU.mult, [pi, cosT], [tmps[2]])
                    P.tt("dve", tv[3], prv, sb3, ALU.mult, [pr, sinT], [tmps[3]])
                    P.tt("pool", gre, tv[0], tv[1], ALU.add, [tmps[0], tmps[1]], [gh])
                    P.tt("pool", gim, tv[2], tv[3], ALU.subtract, [tmps[2], tmps[3]], [gh])
                rb = prm[:, 0, col:col + 1].to_broadcast([128, 128])
                segs = ([0, 1], list(range(2, NTILE))) if d == 0 else ([1, 0], list(range(NTILE - 1, 1, -1)))
                for si, order in enumerate(segs):
                    prev = None
                    for bi_ in order:
                        a = bi_ * 128
                        if d == 0:
                            gsl = lambda t_, ri, a=a: t_[:, ri, a:a + 128]
                            last = lambda t_, ri, a=a: t_[:, ri, a + 127:a + 128]
                        else:
                            gsl = lambda t_, ri, a=a: t_[:, ri, a:a + 128][:, ::-1]
                            last = lambda t_, ri, a=a: t_[:, ri, a:a + 1]
                        Rx = [prm, gh]
                        if prev is None:
                            i_re, i_im = 0.0, 0.0
                        else:
                            it = ini[inic % 2]
                            inic += 1
                            P.ts("dve", it[:, 2:3], prev[0], prm[:, 1, col:col + 1], None, ALU.mult, None, [hh_, prm], [it])
                            P.stt(it[:, 0:1], prev[1], prm[:, 3, col:col + 1], it[:, 2:3], ALU.mult, ALU.add, [hh_, prm, it], [it])
                            P.ts("dve", it[:, 3:4], prev[1], prm[:, 1, col:col + 1], None, ALU.mult, None, [hh_, prm], [it])
                            P.stt(it[:, 1:2], prev[0], prm[:, 2, col:col + 1], it[:, 3:4], ALU.mult, ALU.add, [hh_, prm, it], [it])
                            i_re, i_im = it[:, 0:1], it[:, 1:2]
                            Rx = [prm, gh, it]
                        P.scan(gsl(hh_, 0), rb, gsl(gh, 0), i_re, ALU.mult, ALU.add, Rx, [hh_])
                        P.scan(gsl(hh_, 1), rb, gsl(gh, 1), i_im, ALU.mult, ALU.add, Rx, [hh_])
                        prev = (last(hh_, 0), last(hh_, 1))
                    c127 = cosT[:, col, 127:128]
                    s127 = sinT[:, col, 127:128]
                    P.tt("dve", it4[:, 0:1], prev[0], c127, ALU.mult, [hh_, cosT], [it4])
                    P.tt("dve", it4[:, 1:2], prev[1], s127, ALU.mult, [hh_, sinT], [it4])
                    P.tt("dve", it4[:, 2:3], prev[0], s127, ALU.mult, [hh_, sinT], [it4])
                    P.tt("dve", it4[:, 3:4], prev[1], c127, ALU.mult, [hh_, cosT], [it4])
                    P.tt("dve", ST[:, col, 2 * si:2 * si + 1], it4[:, 0:1], it4[:, 1:2], ALU.subtract, [it4], [ST])
                    P.tt("dve", ST[:, col, 2 * si + 1:2 * si + 2], it4[:, 2:3], it4[:, 3:4], ALU.add, [it4], [ST])
                P.dma("sp", self.HH[col], hh_[:], [hh_], [self.HHb], owner=hh_)
        P.dma("sp", self.XI.ap()[:, 4120:4248], ST[:].rearrange("p c f -> p (c f)"), [ST], [self.XIb], owner=ST)
        P.barrier()
        P.release(m0)

    def stage_s5B(self, l):
        P = self.P
        m0 = P.mark()
        S = self.s5_setup(l, need_B=False)
        self.s5_more_setup(S)
        TinS5 = P.tile([128, 32, 2], F32, "TinS5")
        XG = P.tile([128, NCORE, 128], F32, "s5XG")
        P.dma("sp", XG[:], self.XO.ap()[:, 4120:4248].rearrange("(j p) f -> p j f", p=128), [self.XOb], [XG], owner=XG)
        self.s5_combine(l, S, XG, TinS5)
        cosT, sinT, CEX, prm, dsk, RP, ex = S["cosT"], S["sinT"], S["CEX"], S["prm"], S["dsk"], S["RP"], S["ex"]
        uT = P.tile([128, 4, T], BF16, "s5uT")
        for j in range(4):
            P.dma("sp", uT[:, j, :], self.ZU[j], [self.ZTb], [uT], owner=uT)
        gT = P.tile([128, 4, T], BF16, "s5gT")
        gF = P.tile([128, 4, T], F32, "s5gF")
        Hall = P.tile([128, 8, 2, T], BF16, "s5H")
        hhs = [P.tile([128, 2, T], F32, "s5hh%d" % i) for i in range(2)]
        tmps = [P.tile([128, 512], F32, "s5tm%d" % i) for i in range(4)]
        chs = [P.tile([128, 8], F32, "s5ch%d" % i) for i in range(2)]
        chi = 0

        def rv(ap3, d):
            return ap3 if d == 0 else ap3[:, :, ::-1]

        cnt = 0
        for G4 in range(4):
            for q in range(4):
                gp = G4 * 4 + q
                for d in range(2):
                    col = d * 16 + gp
                    hh_ = hhs[cnt % 2]
                    cnt += 1
                    P.dma("sp", hh_[:], self.HH[col], [self.HHb], [hh_], owner=hh_)
                    ctab = cosT[:, col, :]
                    stab = sinT[:, col, :]
                    ch = chs[chi % 2]
                    chi += 1
                    P.ts("dve", ch[:, 2:3], TinS5[:, col, 0:1], prm[:, 4, col:col + 1], None, ALU.mult, None, [TinS5, prm], [ch])
                    P.stt(ch[:, 0:1], TinS5[:, col, 1:2], prm[:, 6, col:col + 1], ch[:, 2:3], ALU.mult, ALU.add, [TinS5, prm, ch], [ch])
                    P.ts("dve", ch[:, 3:4], TinS5[:, col, 1:2], prm[:, 4, col:col + 1], None, ALU.mult, None, [TinS5, prm], [ch])
                    P.stt(ch[:, 1:2], TinS5[:, col, 0:1], prm[:, 5, col:col + 1], ch[:, 3:4], ALU.mult, ALU.add, [TinS5, prm, ch], [ch])
                    order = list(range(2, NTILE)) if d == 0 else list(range(NTILE - 1, 1, -1))
                    cur = (0, 1)
                    for bi_ in order:
                        a = bi_ * 128
                        for ri in range(2):
                            v = hh_[:, ri, a:a + 128] if d == 0 else hh_[:, ri, a:a + 128][:, ::-1]
                            P.stt(v, RP[:, col, :], ch[:, cur[ri]:cur[ri] + 1], v, ALU.mult, ALU.add, [RP, ch, hh_], [hh_])
                        nxt = (4, 5) if cur == (0, 1) else (0, 1)
                        P.ts("dve", ch[:, 6:7], ch[:, cur[0]:cur[0] + 1], ex[:, 0, col:col + 1], None, ALU.mult, None, [ch, ex], [ch])
                        P.stt(ch[:, nxt[0]:nxt[0] + 1], ch[:, cur[1]:cur[1] + 1], ex[:, 2, col:col + 1], ch[:, 6:7], ALU.mult, ALU.add, [ch, ex], [ch])
                        P.ts("dve", ch[:, 7:8], ch[:, cur[1]:cur[1] + 1], ex[:, 0, col:col + 1], None, ALU.mult, None, [ch, ex], [ch])
                        P.stt(ch[:, nxt[1]:nxt[1] + 1], ch[:, cur[0]:cur[0] + 1], ex[:, 1, col:col + 1], ch[:, 7:8], ALU.mult, ALU.add, [ch, ex], [ch])
                        cur = nxt
                    for (t0, n, wh) in TB:
                        nb = n // 128
                        cb3 = ctab.unsqueeze(1).to_broadcast([128, nb, 128])
                        sb3 = stab.unsqueeze(1).to_broadcast([128, nb, 128])
                        hre = rv(hh_[:, 0, t0:t0 + n].rearrange("p (b j) -> p b j", j=128), d)
                        him = rv(hh_[:, 1, t0:t0 + n].rearrange("p (b j) -> p b j", j=128), d)
                        tv = [rv(tm[:, 0:n].rearrange("p (b j) -> p b j", j=128), d) for tm in tmps]
                        ore = rv(Hall[:, q * 2 + d, 0, t0:t0 + n].rearrange("p (b j) -> p b j", j=128), d)
                        oim = rv(Hall[:, q * 2 + d, 1, t0:t0 + n].rearrange("p (b j) -> p b j", j=128), d)
                        P.tt("dve", tv[0], hre, cb3, ALU.mult, [hh_, cosT], [tmps[0]])
                        P.tt("dve", tv[1], him, sb3, ALU.mult, [hh_, sinT], [tmps[1]])
                        P.tt("dve", tv[2], hre, sb3, ALU.mult, [hh_, sinT], [tmps[2]])
                        P.tt("dve", tv[3], him, cb3, ALU.mult, [hh_, cosT], [tmps[3]])
                        P.tt("pool", ore, tv[0], tv[1], ALU.subtract, [tmps[0], tmps[1]], [Hall])
                        P.tt("pool", oim, tv[2], tv[3], ALU.add, [tmps[2], tmps[3]], [Hall])
            for (t0, n, wh) in TB:
                py = P.ps()
                k = 0
                for q in range(4):
                    for d in range(2):
                        for ri in range(2):
                            P.mm(py[:, 0:n], CEX[:, d, ri, G4 * 4 + q, :], Hall[:, q * 2 + d, ri, t0:t0 + n], k == 0, k == 15, [CEX, Hall], [py])
                            k += 1
                yt = tmps[0]
                P.stt(yt[:, 0:n], uT[:, G4, t0:t0 + n], dsk[:, G4, 0:1], py[:, 0:n], ALU.mult, ALU.add, [uT, dsk, py], [yt])
                P.act(gF[:, G4, t0:t0 + n], yt[:, 0:n], AF.Gelu, [yt], [gF])
                P.copy("pool", gT[:, G4, t0:t0 + n], gF[:, G4, t0:t0 + n], [gF], [gT])
        self.new_wslots_small()
        bgl = P.tile([128, 4, 2], F32, "bglu")
        P.dma("sp", bgl[:, :, 0], self.s5_b_glu[l].rearrange("(j p) -> p j", p=128), (), [bgl], owner=bgl, slow=True)
        stg = [P.tile([128, 512], BF16, "s5stg%d" % i) for i in range(3)]
        sgt = [P.tile([128, 512], F32, "s5sg%d" % i) for i in range(2)]
        cnt2 = [0]

        def evac(j, bi, ps):
            t0, n, wh = TB[bi]
            sg = sgt[cnt2[0] % 2]
            sb = stg[cnt2[0] % 3]
            cnt2[0] += 1
            P.act(sg[:, 0:n], ps[:, 0:n], AF.Sigmoid, [ps, bgl], [sg], bias=bgl[:, j, 0:1])
            P.tt("dve", sb[:, 0:n], sg[:, 0:n], gF[:, j, t0:t0 + n], ALU.mult, [sg, gF], [sb])
            P.dma("sp", self.MIXT[j, :, t0:t0 + n], sb[:, 0:n], [sb], [self.MIXTb], owner=sb)

        self.proj_T(gT, 4, self.s5_w_glu[l], 0, 0, 512, evac)
        P.barrier()
        P.release(m0)

    def s5_combine(self, l, S, XG, TinS5):
        P = self.P
        ex = S["ex"]
        fl = P.tile([128, 16], F32, "s5flags")
        P.dma("sp", fl[:], self.cflags, (), [fl], owner=fl)
        m1 = P.mark()
        STl = P.tile([128, 32, 4], F32, "s5STl")
        P.dma("sp", STl[:].rearrange("p c f -> p (c f)"), self.XI.ap()[:, 4120:4248], [self.XIb], [STl], owner=STl)
        tr = P.tile([128, 16], F32, "s5c_tr")
        ti = P.tile([128, 16], F32, "s5c_ti")
        na = P.tile([128, 16], F32, "s5c_na")
        nb_ = P.tile([128, 16], F32, "s5c_nb")
        for d in range(2):
            cs_ = slice(d * 16, (d + 1) * 16)
            P.copy("dve", tr[:], STl[:, cs_, 0], [STl], [tr])
            P.copy("dve", ti[:], STl[:, cs_, 1], [STl], [ti])
            Lr, Li = ex[:, 3, cs_], ex[:, 4, cs_]
            for j in (range(NCORE) if d == 0 else range(NCORE - 1, -1, -1)):
                xg = XG[:, j, :].rearrange("p (c f) -> p c f", f=4)
                hr, hi = xg[:, cs_, 2], xg[:, cs_, 3]
                f = fl[:, d * 8 + j:d * 8 + j + 1]
                P.tt("dve", na[:], tr[:], Lr, ALU.mult, [tr, ex], [na])
                P.tt("dve", nb_[:], ti[:], Li, ALU.mult, [ti, ex], [nb_])
                P.tt("dve", na[:], na[:], nb_[:], ALU.subtract, [na, nb_], [na])
                P.tt("dve", na[:], na[:], hr, ALU.add, [na, XG], [na])
                P.tt("dve", nb_[:], tr[:], Li, ALU.mult, [tr, ex], [nb_])
                P.tt("dve", na[:], na[:], tr[:], ALU.subtract, [na, tr], [na])
                P.stt(tr[:], na[:], f, tr[:], ALU.mult, ALU.add, [na, fl, tr], [tr])
                P.tt("dve", na[:], ti[:], Lr, ALU.mult, [ti, ex], [na])
                P.tt("dve", na[:], na[:], nb_[:], ALU.add, [na, nb_], [na])
                P.tt("dve", na[:], na[:], hi, ALU.add, [na, XG], [na])
                P.tt("dve", na[:], na[:], ti[:], ALU.subtract, [na, ti], [na])
                P.stt(ti[:], na[:], f, ti[:], ALU.mult, ALU.add, [na, fl, ti], [ti])
            P.copy("dve", TinS5[:, cs_, 0], tr[:], [tr], [TinS5])
            P.copy("dve", TinS5[:, cs_, 1], ti[:], [ti], [TinS5])
        P.release(m1)

    def new_wslots_small(self):
        self.wslots = [self.P.tile([128, 4, 512], BF16, "wslotS%d" % i) for i in range(2)]

    def ml_setup(self, l):
        P = self.P
        S = {}
        c16 = P.tile([16, 24], F32, "c16")
        P.dma("sp", c16[:], self.c16, (), [c16], owner=c16)
        c4 = P.tile([4, 4, 128], F32, "c4")
        P.dma("sp", c4[:], self.c4, (), [c4], owner=c4)
        c128 = P.tile([128, 5, 128], F32, "c128")
        P.dma("sp", c128[:], self.c128, (), [c128], owner=c128)
        S["c4"], S["c128"] = c4, c128
        AZ = [P.tile([4, T + 2], F32, "AZ%d" % d) for d in range(2)]
        AT = P.tile([128, NTILE, 8, 2], F32, "AT")
        BT = P.tile([128, NTILE, 8, 2], F32, "BT")
        S["REFZ"] = P.tile([128, 8, 11, 2], F32, "REFZ")
        m1 = P.mark()
        G = P.tile([16, T], F32, "mlG")
        P.dma("sp", G[:], self.ZG, [self.ZTb], [G], owner=G)
        L1 = P.tile([16, T], F32, "mlL1")
        P.act(L1[:], G[:], AF.Exp, [G], [L1], scale=-1.0)
        P.act(L1[:], L1[:], AF.Ln, [L1, self.one_c], [L1], bias=self.one_c[0:16, 0:1])
        one16 = P.tile([16, T], F32, "one16")
        P.memset("dve", one16[:], 1.0, [one16])
        FZ = P.tile([16, T + 1], F32, "mlFZ")
        P.memset("dve", FZ[:, 0:1], 0.0, [FZ])
        P.scan(FZ[:, 1:T + 1], one16[:], L1[:], 0.0, ALU.mult, ALU.add, [one16, L1], [FZ])
        BE = [P.tile([4, T], F32, "BE%d" % d) for d in range(2)]
        P.memset("dve", AZ[0][:], 0.0, [AZ[0]])
        P.memset("dve", AZ[1][:], 0.0, [AZ[1]])
        blocks = [(0, 512), (512, 512), (1024, 257)]
        for (u0, n) in blocks:
            ps = P.ps()
            P.mm(ps[0:4, 0:n], c16[:, 12:16], FZ[:, u0:u0 + n], True, True, [c16, FZ], [ps])
            P.copy("dve", AZ[0][:, u0:u0 + n], ps[0:4, 0:n], [ps], [AZ[0]])
            ps = P.ps()
            P.mm(ps[0:4, 0:n], c16[:, 16:20], FZ[:, u0:u0 + n], True, True, [c16, FZ], [ps])
            P.copy("dve", AZ[1][:, 1 + u0:1 + u0 + n], ps[0:4, 0:n], [ps], [AZ[1]])
        for (u0, n) in [(0, 512), (512, 512), (1024, 256)]:
            ps = P.ps()
            P.mm(ps[0:4, 0:n], c16[:, 0:4], G[:, u0:u0 + n], True, False, [c16, G], [ps])
            P.mm(ps[0:4, 0:n], c16[:, 8:12], FZ[:, 1 + u0:1 + u0 + n], False, True, [c16, FZ], [ps])
            P.copy("dve", BE[0][:, u0:u0 + n], ps[0:4, 0:n], [ps], [BE[0]])
            ps = P.ps()
            P.mm(ps[0:4, 0:n], c16[:, 4:8], G[:, u0:u0 + n], True, False, [c16, G], [ps])
            P.mm(ps[0:4, 0:n], c16[:, 20:24], FZ[:, u0:u0 + n], False, True, [c16, FZ], [ps])
            P.copy("dve", BE[1][:, u0:u0 + n], ps[0:4, 0:n], [ps], [BE[1]])
        ident4 = c128[0:4, 2, 0:4]
        for d in range(2):
            for i in range(NTILE):
                ps = P.ps()
                P.mm(ps[:, 0:4], AZ[d][:, 1 + i * 128:1 + (i + 1) * 128], ident4, True, True, [AZ[d], c128], [ps])
                P.mm(ps[:, 4:8], BE[d][:, i * 128:(i + 1) * 128], ident4, True, True, [BE[d], c128], [ps])
                P.copy("dve", AT[:, i, d * 4:(d + 1) * 4, 0], ps[:, 0:4], [ps], [AT])
                P.copy("dve", BT[:, i, d * 4:(d + 1) * 4, 0], ps[:, 4:8], [ps], [BT])
        REFZ = S["REFZ"]
        for d in range(2):
            for h in range(4):
                c = d * 4 + h
                for (u0, n, i0, ni) in [(0, 512, 0, 4), (512, 512, 4, 4), (1024, 258, 8, 2)]:
                    ps = P.ps()
                    P.mm(ps[:, 0:n], c4[:, h, :], AZ[d][:, u0:u0 + n], True, True, [c4, AZ[d]], [ps])
                    P.copy("dve", REFZ[:, c, i0:i0 + ni, :], ps[:, 0:ni * 128].rearrange("p (i r) -> p i r", r=128)[:, :, 0:2], [ps], [REFZ])
                    if u0 == 1024:
                        P.copy("dve", REFZ[:, c, 10, :], ps[:, 256:258], [ps], [REFZ])
        S["AZ"], S["AT"], S["BT"] = AZ, AT, BT
        P.release(m1)
        return S

    def ml_chunk(self, S, d, h, i, main):
        P = self.P
        c = d * 4 + h
        a = i * 128
        AT, BT, REFZ, AZ, c4, c128 = S["AT"], S["BT"], S["REFZ"], S["AZ"], S["c4"], S["c128"]
        ZQs, ZKs, Vx, HS = S["ZQs"], S["ZKs"], S["Vx"], S["HS"]
        Cst, Cb = S["Cst"][c], S["Cb"][c]
        Kt = S["Ktc"][i]
        if d == 0:
            z_in, z_e, z1, z2 = REFZ[:, c, i, 0:1], REFZ[:, c, i + 1, 0:1], REFZ[:, c, i, 0:1], REFZ[:, c, i + 1, 0:1]
        else:
            z_in, z_e, z1, z2 = REFZ[:, c, i + 1, 1:2], REFZ[:, c, i, 1:2], REFZ[:, c, i + 1, 1:2], REFZ[:, c, i, 1:2]
        k_ = S["sci"]
        S["sci"] += 1
        sc = S["sc"][k_ % 8]
        P.tt("dve", sc[:, 0:1], AT[:, i, c, 0:1], z_in, ALU.subtract, [AT, REFZ], [sc])
        P.tt("dve", sc[:, 1:2], BT[:, i, c, 0:1], z_e, ALU.add, [BT, REFZ], [sc])
        P.tt("dve", sc[:, 2:3], z2, z1, ALU.subtract, [REFZ], [sc])
        P.act(sc[:, 4:7], sc[:, 0:3], AF.Exp, [sc], [sc])
        wj, es, dec = sc[:, 4:5], sc[:, 5:6], sc[:, 6:7]
        if main:
            ps_d = P.ps()
            P.mm(ps_d[:, 0:128], c4[:, h, :], AZ[d][:, a + 1:a + 129], True, True, [c4, AZ[d]], [ps_d])
            ps_st = P.ps()
            for kt in range(2):
                P.mm(ps_st[:, 0:128], ZKs[:, 2 * h + kt, a:a + 128], ZQs[:, 2 * h + kt, a:a + 128], kt == 0, kt == 1, [ZKs, ZQs], [ps_st])
            Dt = S["Dt"][k_ % 4]
            P.act(Dt[:], ps_d[:, 0:128], AF.Exp, [ps_d, BT], [Dt], bias=BT[:, i, c, 0:1])
            Wt = S["Wt"][k_ % 4]
            P.tt("dve", Dt[:], ps_st[:, 0:128], Dt[:], ALU.mult, [ps_st, Dt], [Dt])
            P.tt("pool", Wt[:], Dt[:], c128[:, 3 + d, :], ALU.mult, [Dt, c128], [Wt])
            ps_o = P.ps()
            P.mm(ps_o[:, 0:257], Wt[:], Vx[:, i, h, :], True, True, [Wt, Vx], [ps_o])
            ps_i = P.ps()
            for kt in range(2):
                P.mm(ps_i[:, 0:257], ZQs[:, 2 * h + kt, a:a + 128], Cb[:, kt, :], kt == 0, kt == 1, [ZQs, Cb], [ps_i])
            tmp = S["tmp"][k_ % 4]
            P.copy("act", tmp[:], ps_o[:, 0:257], [ps_o], [tmp])
            P.stt(tmp[:], ps_i[:, 0:257], wj, tmp[:], ALU.mult, ALU.add, [ps_i, sc, tmp], [tmp])
            P.act(sc[:, 8:9], tmp[:, 256:257], AF.Abs, [tmp], [sc])
            P.ts("dve", sc[:, 8:9], sc[:, 8:9], 1.0, None, ALU.max, None, [sc], [sc])
            P.recip(sc[:, 9:10], sc[:, 8:9], [sc], [sc])
            hsl = HS[:, i, h * 256:(h + 1) * 256]
            P.stt(hsl, tmp[:, 0:256], sc[:, 9:10], hsl, ALU.mult, ALU.add, [tmp, sc, S["HSb"][i][h]], [S["HSb"][i][h]])
        kw = S["kw"][k_ % 4]
        P.ts("pool", kw[:], Kt[:, h * 256:(h + 1) * 256], es, None, ALU.mult, None, [Kt, sc], [kw])
        for kt in range(2):
            ps_s = P.ps()
            P.mm(ps_s[:, 0:257], kw[:, kt * 128:(kt + 1) * 128], Vx[:, i, h, :], True, True, [kw, Vx], [ps_s])
            P.stt(Cst[:, kt, :], Cst[:, kt, :], dec, ps_s[:, 0:257], ALU.mult, ALU.add, [Cst, sc, ps_s], [Cst])
        P.copy("act", Cb[:], Cst[:], [Cst], [Cb])

    def ml_steps(self, S, pairs, main):
        P = self.P
        if os.environ.get("ML_SEQ"):
            for d in range(2):
                for h in range(4):
                    for (fi, bi_) in pairs:
                        i = fi if d == 0 else bi_
                        kt_ = S["Ktbuf"][S["Kti"] % len(S["Ktbuf"])]
                        S["Kti"] += 1
                        P.dma("sp", kt_[:], self.KTOK[i * 128:(i + 1) * 128, :], [self.ZTb], [kt_], owner=kt_)
                        S["Ktc"][i] = kt_
                        self.ml_chunk(S, d, h, i, main)
            S["Ktc"].clear()
            return
        for (fi, bi_) in pairs:
            for i in sorted(set((fi, bi_))):
                if i not in S["Ktc"]:
                    kt_ = S["Ktbuf"][S["Kti"] % len(S["Ktbuf"])]
                    S["Kti"] += 1
                    P.dma("sp", kt_[:], self.KTOK[i * 128:(i + 1) * 128, :], [self.ZTb], [kt_], owner=kt_)
                    S["Ktc"][i] = kt_
            for h in range(4):
                self.ml_chunk(S, 0, h, fi, main)
                self.ml_chunk(S, 1, h, bi_, main)
            S["Ktc"].clear()

    def stage_exchange(self):
        P = self.P
        if self.ncores == 1:
            for j in range(NCORE):
                P.dma("sp", self.XO.ap()[j * 128:(j + 1) * 128, :], self.XI.ap()[:, :], [self.XIb], [self.XOb], owner=self.XOb)
        else:
            xi, xo = self.XI, self.XO
            P.op("pool", lambda h: h.collective_compute("AllGather", ALU.bypass, replica_groups=[list(range(NCORE))],
                                                        ins=[xi.ap().opt()], outs=[xo.ap().opt()]), [self.XIb], [self.XOb])
        P.barrier()

    def stage_mlstm(self, l):
        P = self.P
        need_ctx = l < DEPTH - 1
        m0 = P.mark()
        S = self.ml_setup(l)
        S["HS"] = P.tile([128, NTILE, 1024], F32, "HS")
        S["HSb"] = [[Buf("HS%d_%d" % (i, h)) for h in range(4)] for i in range(NTILE)]
        for i in range(NTILE):
            P.memset("pool", S["HS"][:, i, :], 0.0, [S["HS"]] + [S["HSb"][i][h] for h in range(4)])
        mph = P.mark()
        S["ZQs"] = P.tile([128, 8, T], BF16, "ZQs")
        S["ZKs"] = P.tile([128, 8, T], BF16, "ZKs")
        for j in range(8):
            P.dma("sp", S["ZQs"][:, j, :], self.ZQ[j], [self.ZTb], [S["ZQs"]], owner=S["ZQs"])
            P.dma("act", S["ZKs"][:, j, :], self.ZK[j], [self.ZTb], [S["ZKs"]], owner=S["ZKs"])
        S["Vx"] = P.tile([128, NTILE, 4, 257], BF16, "Vx")
        P.memset("dve", S["Vx"][:], 1.0, [S["Vx"]])
        for i in range(NTILE):
            P.dma("act", S["Vx"][:, i, :, 0:256], self.VTOK[i * 128:(i + 1) * 128, :].rearrange("p (h v) -> p h v", v=256),
                  [self.ZTb], [S["Vx"]], owner=S["Vx"])
        S["Ktbuf"] = [P.tile([128, 1024], BF16, "Ktb%d" % k) for k in range(4)]
        S["Kti"] = 0
        S["Ktc"] = {}
        S["sc"] = [P.tile([128, 12], F32, "mlsc%d" % k) for k in range(8)]
        S["sci"] = 0
        S["Dt"] = [P.tile([128, 128], F32, "Dt%d" % k) for k in range(4)]
        S["Wt"] = [P.tile([128, 128], BF16, "Wt%d" % k) for k in range(4)]
        S["tmp"] = [P.tile([128, 257], F32, "mltmp%d" % k) for k in range(4)]
        S["kw"] = [P.tile([128, 256], BF16, "kw%d" % k) for k in range(4)]
        S["Cst"] = [P.tile([128, 2, 257], F32, "Cst%d" % c) for c in range(8)]
        S["Cb"] = [P.tile([128, 2, 257], BF16, "Cb%d" % c) for c in range(8)]
        bt = P.tile([128, 8], F32, "mlBtot")
        REFZ = S["REFZ"]
        XI, XO = self.XI.ap(), self.XO.ap()

        def zero_states():
            for c in range(8):
                P.memset("dve", S["Cst"][c][:], 0.0, [S["Cst"][c]])
                P.memset("pool", S["Cb"][c][:], 0.0, [S["Cb"][c]])

        zero_states()
        self.ml_steps(S, [(0, 1), (1, 0)], True)
        for c in range(8):
            P.dma("sp", self.SCD[:, c, :], S["Cst"][c][:].rearrange("p k f -> p (k f)"), [S["Cst"][c]], [self.SCDb], owner=S["Cst"][c])
        zero_states()
        self.ml_steps(S, [(2 + st, NTILE - 1 - st) for st in range(8)], False)
        for c in range(8):
            P.dma("sp", XI[:, c * 514:(c + 1) * 514], S["Cst"][c][:].rearrange("p k f -> p (k f)"), [S["Cst"][c]], [self.XIb], owner=S["Cst"][c])
            if c < 4:
                P.tt("dve", bt[:, c:c + 1], REFZ[:, c, 10, 0:1], REFZ[:, c, 2, 0:1], ALU.subtract, [REFZ], [bt])
            else:
                P.tt("dve", bt[:, c:c + 1], REFZ[:, c, 2, 1:2], REFZ[:, c, 10, 1:2], ALU.subtract, [REFZ], [bt])
        P.dma("sp", XI[:, 4112:4120], bt[:], [bt], [self.XIb], owner=bt)
        self.stage_exchange()
        fl = P.tile([128, 16], F32, "mlflags")
        P.dma("sp", fl[:], self.cflags, (), [fl], owner=fl)
        Bg = P.tile([128, NCORE, 8], F32, "mlBg")
        P.dma("sp", Bg[:], XO[:, 4112:4120].rearrange("(j p) f -> p j f", p=128), [self.XOb], [Bg], owner=Bg)
        Aw = P.tile([128, NCORE, 8], F32, "mlAw")
        P.act(Aw[:], Bg[:], AF.Exp, [Bg], [Aw])
        P.ts("dve", Aw[:], Aw[:], -1.0, None, ALU.add, None, [Aw], [Aw])
        for d in range(2):
            P.tt("dve", Aw[:, :, d * 4:(d + 1) * 4], Aw[:, :, d * 4:(d + 1) * 4],
                 fl[:, d * 8:(d + 1) * 8].unsqueeze(2).to_broadcast([128, NCORE, 4]), ALU.mult, [Aw, fl], [Aw])
        P.ts("dve", Aw[:], Aw[:], 1.0, None, ALU.add, None, [Aw], [Aw])
        sg = [P.tile([128, 514], F32, "mlSG%d" % k) for k in range(3)]
        sgi = 0
        for d in range(2):
            for h in range(4):
                c = d * 4 + h
                Cst, Cb = S["Cst"][c], S["Cb"][c]
                Cf = Cst[:].rearrange("p k f -> p (k f)")
                P.dma("sp", Cf, self.SCD[:, c, :], [self.SCDb], [Cst], owner=Cst)
                for j in (range(NCORE) if d == 0 else range(NCORE - 1, -1, -1)):
                    g_ = sg[sgi % 3]
                    sgi += 1
                    P.dma("act", g_[:], XO[j * 128:(j + 1) * 128, c * 514:(c + 1) * 514], [self.XOb], [g_], owner=g_)
                    P.ts("pool", g_[:], g_[:], fl[:, d * 8 + j:d * 8 + j + 1], None, ALU.mult, None, [g_, fl], [g_])
                    P.stt(Cf, Cf, Aw[:, j, c:c + 1], g_[:], ALU.mult, ALU.add, [Cst, Aw, g_], [Cst])
                P.copy("act", Cb[:], Cst[:], [Cst], [Cb])
        self.ml_steps(S, [(2 + st, NTILE - 1 - st) for st in range(8)], True)
        P.barrier()
        P.release(mph)
        mlg = P.tile([128, 1024], F32, "mlg")
        P.dma("sp", mlg[:], self.ml_norm_g[l].partition_broadcast(128), (), [mlg], owner=mlg, slow=True)
        HS = S["HS"]
        identf = S["c128"][:, 2, :]
        stg = [P.tile([128, 128], BF16, "mstg%d" % k) for k in range(3)]
        cnt = 0
        for i in range(NTILE if need_ctx else NTILE):
            m1 = P.mark()
            ot = P.tile([128, 1024], F32, "mlO")
            P.dma("sp", ot[:], self.OTOK[i * 128:(i + 1) * 128, :], [self.ZTb], [ot], owner=ot)
            hs = HS[:, i, :].rearrange("p (h v) -> p h v", v=256)
            st = P.tile([128, 8], F32, "mlst")
            P.op("dve", lambda hh, st=st, hs=hs: hh.tensor_reduce(out=st[:, 0:4], in_=hs, axis=AX.X, op=ALU.add), [HS], [st])
            P.ts("dve", st[:, 0:4], st[:, 0:4], 1.0 / 256.0, None, ALU.mult, None, [st], [st])
            cen = P.tile([128, 4, 256], F32, "mlcen")
            P.tt("dve", cen[:], hs, st[:, 0:4].unsqueeze(2).to_broadcast([128, 4, 256]), ALU.subtract, [HS, st], [cen])
            sq = P.tile([128, 4, 256], F32, "mlsq")
            P.act(sq[:], cen[:], AF.Square, [cen], [sq])
            P.op("dve", lambda hh, st=st, sq=sq: hh.tensor_reduce(out=st[:, 4:8], in_=sq[:], axis=AX.X, op=ALU.add), [sq], [st])
            rs = P.tile([128, 4], F32, "mlrs")
            self.rsqrt(rs, st[:, 4:8], 4, 1.0 / 256.0, self.lneps_c, [st])
            P.tt("dve", cen[:], cen[:], rs[:].unsqueeze(2).to_broadcast([128, 4, 256]), ALU.mult, [cen, rs], [cen])
            cf = cen[:].rearrange("p h v -> p (h v)")
            P.tt("dve", cf, cf, mlg[:], ALU.mult, [cen, mlg], [cen])
            P.tt("dve", cf, cf, ot[:], ALU.mult, [cen, ot], [cen])
            for cb in range(8):
                ps = P.ps()
                P.mm(ps[:, 0:128], cen[:].rearrange("p h v -> p (h v)")[:, cb * 128:(cb + 1) * 128], identf, True, True, [cen, S["c128"]], [ps])
                sb = stg[cnt % 3]
                cnt += 1
                P.copy("act", sb[:], ps[:, 0:128], [ps], [sb])
                P.dma("sp", self.MIXT[8 + cb, :, i * 128:(i + 1) * 128], sb[:], [sb], [self.MIXTb], owner=sb)
            P.release(m1)
        P.barrier()
        P.release(m0)

    def stage_wout(self, l):
        P = self.P
        m0 = P.mark()
        self.new_wslots()
        mt = P.tile([128, KT, T], BF16, "mixT")
        for kt in range(KT):
            P.dma("sp" if kt % 2 == 0 else "act", mt[:, kt, :], self.MIXT[kt], [self.MIXTb], [mt], owner=mt)
        self.resid_proj(mt, KT, self.w_out[l], 0, 2)
        P.barrier()
        P.release(m0)

    def resid_proj(self, hT, nk, wsrc, r0, gate_v):
        P = self.P
        xs = [P.tile([128, 512], F32, "xrmw%d" % i) for i in range(4)]
        cnt = [0]
        cur = {}

        def pre(j, bi):
            t0, n, wh = TB[bi]
            s = xs[cnt[0] % 4]
            cnt[0] += 1
            cur[(j, bi)] = s
            P.dma("act", s[:, 0:n], self.XT[j, :, t0:t0 + n], [self.XTb[(j, bi)]], [s], owner=s)

        def evac(j, bi, ps):
            t0, n, wh = TB[bi]
            s = cur.pop((j, bi))
            xb = self.XTb[(j, bi)]
            P.stt(s[:, 0:n], ps[:, 0:n], self.mod[:, gate_v * KT + j, wh, 0:1], s[:, 0:n], ALU.mult, ALU.add,
                  [ps, self.mod, s], [s])
            P.dma("sp", self.XT[j, :, t0:t0 + n], s[:, 0:n], [s], [xb], owner=s)

        self.proj_T(hT, nk, wsrc, r0, 0, D, evac, pre)

    def stage_ffn(self, l):
        P = self.P
        m0 = P.mark()
        self.new_wslots()
        h2 = P.tile([128, KT, T], BF16, "h2T")
        self.stage_norm(h2, self.gm2, 3)
        FCH = 1024
        nch = (D_FF + FCH - 1) // FCH
        for ch in range(nch):
            m1 = P.mark()
            f0 = ch * FCH
            fw = min(FCH, D_FF - f0)
            nft = fw // 128
            actT = P.tile([128, nft, T], BF16, "actT")
            gtmp = [P.tile([128, 512], F32, "gtmp%d" % i) for i in range(2)]
            gi = [0]
            for cb in range(fw // 512):
                wg = self.load_w(self.w_ffn_in[l], 0, KT, f0 + cb * 512, 512)
                wu = self.load_w(self.w_ffn_in[l], 0, KT, D_FF + f0 + cb * 512, 512)
                for jj in range(4):
                    ft = cb * 4 + jj
                    for bi, (t0, n, wh) in enumerate(TB):
                        pg = P.ps()
                        pu = P.ps()
                        for kt in range(KT):
                            P.mm(pg[:, 0:n], wg[:, kt, jj * 128:(jj + 1) * 128], h2[:, kt, t0:t0 + n], kt == 0, kt == KT - 1, [wg, h2], [pg])
                        for kt in range(KT):
                            P.mm(pu[:, 0:n], wu[:, kt, jj * 128:(jj + 1) * 128], h2[:, kt, t0:t0 + n], kt == 0, kt == KT - 1, [wu, h2], [pu])
                        g = gtmp[gi[0] % 2]
                        gi[0] += 1
                        P.act(g[:, 0:n], pg[:, 0:n], AF.Silu, [pg], [g])
                        P.tt("dve", actT[:, ft, t0:t0 + n], g[:, 0:n], pu[:, 0:n], ALU.mult, [g, pu], [actT])
            self.resid_proj(actT, nft, self.w_ffn_out[l], f0, 5)
            P.barrier()
            P.release(m1)
        P.release(m0)

    def stage_final(self):
        P = self.P
        m0 = P.mark()
        gf = P.tile([128, KT], F32, "gf")
        P.dma("sp", gf[:], self.norm_f_g.rearrange("(j p) -> p j", p=128), (), [gf], owner=gf, slow=True)
        for bi, (t0, n, wh) in enumerate(TB):
            if wh == 1:
                continue
            m1 = P.mark()
            xt = P.tile([128, KT, n], F32, "fx")
            for kt in range(KT):
                P.dma("sp" if kt % 2 == 0 else "act", xt[:, kt, :], self.XT[kt, :, t0:t0 + n], [self.XTb[(kt, bi)]], [xt], owner=xt)
            sq = P.tile([128, KT, n], BF16, "fsq")
            P.act(sq[:], xt[:], AF.Square, [xt], [sq])
            ps = P.ps()
            for kt in range(KT):
                P.mm(ps[:, 0:n], self.ones_bf[:], sq[:, kt, :], kt == 0, kt == KT - 1, [sq, self.ones_bf], [ps])
            rstd = P.tile([128, n], F32, "frstd")
            self.rsqrt(rstd, ps[:, 0:n], n, 1.0 / D, self.eps_c, [ps])
            ot = P.tile([128, KT, n], F32, "fo")
            for kt in range(KT):
                P.stt(ot[:, kt, :], xt[:, kt, :], gf[:, kt:kt + 1], rstd[:], ALU.mult, ALU.mult, [xt, gf, rstd], [ot])
            for kt in range(KT):
                P.dma("sp" if kt % 2 == 0 else "act", self.outT[kt * 128:(kt + 1) * 128, t0 - CTX:t0 - CTX + n], ot[:, kt, :], [ot], (), owner=ot)
            P.barrier()
            P.release(m1)
        P.release(m0)

    def stage_mix_stub(self):
        P = self.P
        m0 = P.mark()
        z = P.tile([128, T], BF16, "zeros")
        P.memset("dve", z[:], 0.0, [z])
        for kt in range(KT):
            P.dma("sp", self.MIXT[kt], z[:], [z], [self.MIXTb], owner=z)
        P.barrier()
        P.release(m0)

    def build(self, mixers=True):
        P = self.P
        self.setup_consts()
        self.stage_load_x()
        for l in range(self.nlayers):
            self.stage_mod(l)
            m0 = P.mark()
            self.new_wslots()
            h1 = P.tile([128, KT, T], BF16, "h1T")
            self.stage_norm(h1, self.gm1, 0)
            self.dump("d_h1_%d" % l, h1, [128, KT, T], BF16)
            self.stage_proj_in(l, h1)
            self.stage_proj_tok(l, h1)
            P.release(m0)
            self.XI, self.XO = self.XIs[l], self.XOs[l]
            self.stage_conv(l)
            self.stage_s5A(l)
            self.stage_mlstm(l)
            self.stage_s5B(l)
            self.stage_wout(l)
            self.stage_ffn(l)
        self.stage_final()
        P.emit()
        return self.nc


def host_consts():
    c16 = np.zeros((16, 24), np.float32)
    for m in range(4):
        c16[m, 0 + m] = 1.0
        c16[8 + m, 4 + m] = 1.0
        c16[4 + m, 8 + m] = 1.0
        c16[4 + m, 12 + m] = -1.0
        c16[12 + m, 16 + m] = 1.0
        c16[12 + m, 20 + m] = -1.0
    c128 = np.zeros((128, 5, 128), np.float32)
    sidx = np.arange(128)[:, None]
    jidx = np.arange(128)[None, :]
    c128[:, 0, :] = np.where(sidx <= jidx, 0.0, -30000.0)
    c128[:, 1, :] = np.where(sidx >= jidx, 0.0, -30000.0)
    c128[:, 2, :] = np.eye(128, dtype=np.float32)
    c128[:, 3, :] = (sidx <= jidx).astype(np.float32)
    c128[:, 4, :] = (sidx >= jidx).astype(np.float32)
    c4 = np.zeros((4, 4, 128), np.float32)
    for h in range(4):
        c4[h, h, :] = 1.0
    return {"c16": c16, "c128": c128, "c4": c4}


WKEYS = ["s5_lam_re", "s5_lam_im", "s5_log_step", "s5_b_re", "s5_b_im", "s5_c_re", "s5_c_im", "s5_d", "s5_w_glu", "s5_b_glu",
         "conv_dw_w", "conv_dw_b", "conv_ln_g", "conv_ln_b", "ml_norm_g", "w_mod", "b_mod", "norm1_g", "w_in", "b_in", "w_out", "norm2_g", "w_ffn_in", "w_ffn_out", "norm_f_g"]


def make_in_maps(inputs):
    x = np.asarray(inputs["x"], np.float32)[0]
    ctx = np.asarray(inputs["ctx"], np.float32)[0]
    ctxT = np.ascontiguousarray(ctx.T)
    cc = np.ascontiguousarray(np.stack([np.asarray(inputs["c"], np.float32)[0], np.asarray(inputs["c_ctx"], np.float32)], axis=1))
    shared = {k: np.ascontiguousarray(np.asarray(inputs[k], np.float32)) for k in WKEYS}
    shared.update(host_consts())
    maps = []
    for c in range(x.shape[0] // XTOK):
        m = dict(shared)
        fl = np.zeros((128, 16), np.float32)
        fl[:, 0:8] = (np.arange(8) < c).astype(np.float32)[None, :]
        fl[:, 8:16] = (np.arange(8) > c).astype(np.float32)[None, :]
        m["cflags"] = fl
        m["xT"] = np.ascontiguousarray(x[c * XTOK:(c + 1) * XTOK].T)
        m["ctxT"] = ctxT
        m["cc"] = cc
        maps.append(m)
    return maps


def kernel(**inputs):
    k = K()
    nc = k.build()
    maps = make_in_maps(inputs)
    res = run_bass_kernel_spmd(nc, maps, core_ids=list(range(NCORE)))
    outs = [np.asarray(r["outT"]).T for r in res.results]
    return np.concatenate(outs, axis=0)[None].astype(np.float32)
```

```python
import os
import numpy as np
import concourse.bass as bass
import concourse.mybir as mybir
from concourse.bass_utils import run_bass_kernel_spmd

F32 = mybir.dt.float32
BF16 = mybir.dt.bfloat16
AF = mybir.ActivationFunctionType
ALU = mybir.AluOpType
AX = mybir.AxisListType

D = 2048
KT = 16
DEPTH = 4
SEQ = 8192
NCORE = 8
XTOK = 1024
CTX = 256
T = CTX + XTOK
NTILE = T // 128
IN_W = 5648
D_FF = 5632
EPS = 1e-6
LN_EPS = 1e-5
TB = [(0, 256, 1), (256, 512, 0), (768, 512, 0)]

ARENA_LO = 16640
ARENA_HI = 229376


def _prod(xs):
    r = 1
    for v in xs:
        r *= int(v)
    return r


class Buf:
    __slots__ = ("name", "w", "r", "so")

    def __init__(self, name=""):
        self.name = name
        self.w = None
        self.r = {}
        self.so = None


class SemObj:
    __slots__ = ("sem", "tot", "owner")

    def __init__(self, sem):
        self.sem = sem
        self.tot = 0
        self.owner = None


class Tl:
    def __init__(self, t, b):
        self.t = t
        self.b = b

    def __getitem__(self, k):
        return self.t[k]


def _b(x):
    return x.b if isinstance(x, Tl) else x


class Prog:
    ENG = ("pe", "act", "dve", "pool", "sp")
    CE = ("pe", "act", "dve", "pool")

    def __init__(self, nc):
        self.nc = nc
        self.q = {e: [] for e in self.ENG}
        self.sem = {e: nc.alloc_semaphore("s_" + e) for e in self.CE}
        self.cnt = {e: 0 for e in self.CE}
        self.seen = {e: {} for e in self.ENG}
        self.pool = [SemObj(nc.alloc_semaphore("d%d" % i)) for i in range(80)]
        self.pool_i = 0
        self.top = ARENA_LO
        self.nalloc = 0
        self.regions = []
        self.psum = [Tl(nc.alloc_psum_tensor("psb%d" % i, [128, 512], F32), Buf("ps%d" % i)) for i in range(8)]
        self.psi = 0

    def tile(self, shape, dtype, name="t"):
        nbytes = _prod(shape[1:]) * mybir.dt.size(dtype)
        nbytes = (nbytes + 63) // 64 * 64
        off = self.top
        self.top += nbytes
        assert self.top <= ARENA_HI, "SBUF arena overflow %d" % self.top
        self.nalloc += 1
        nm = "%s_%d" % (name, self.nalloc)
        nb = Buf(nm)
        lo, hi = off, off + nbytes
        keep = []
        for (a, b_, ob) in self.regions:
            if a < hi and lo < b_:
                if ob.w is not None:
                    nb.r[("inh", len(nb.r))] = ob.w
                for tok in ob.r.values():
                    nb.r[("inh", len(nb.r))] = tok
                if not (lo <= a and b_ <= hi):
                    keep.append((a, b_, ob))
            else:
                keep.append((a, b_, ob))
        keep.append((lo, hi, nb))
        self.regions = keep
        return Tl(self.nc.alloc_sbuf_tensor_at(nm, list(shape), dtype, offset=off), nb)

    def mark(self):
        return self.top

    def release(self, m):
        self.top = m

    def dram(self, name, shape, dtype, kind="Internal"):
        return self.nc.dram_tensor(name, list(shape), dtype, kind=kind).ap()

    def ps(self):
        p = self.psum[self.psi % 8]
        self.psi += 1
        return p

    def _wait(self, st, key, sem, val):
        if self.seen[st].get(key, 0) >= val:
            return
        self.seen[st][key] = val
        self.q[st].append(lambda h, sem=sem, val=val: h.wait_ge(sem, val))

    def _deps(self, st, R, W):
        toks = []
        for b in R:
            if b.w is not None:
                toks.append(b.w)
        for b in W:
            if b.w is not None:
                toks.append(b.w)
            toks.extend(b.r.values())
        for kind, obj, val in toks:
            if kind == "e":
                if obj == st and st == "pe":
                    continue
                self._wait(st, obj, self.sem[obj], val)
            else:
                self._wait(st, id(obj), obj.sem, obj.tot)

    def _post(self, tok, key, R, W):
        for b in W:
            b.w = tok
            b.r = {}
        for b in R:
            if b not in W:
                b.r[key] = tok

    def op(self, eng, fn, R=(), W=()):
        R = [_b(x) for x in R]
        W = [_b(x) for x in W]
        self._deps(eng, R, W)
        self.cnt[eng] += 1
        sem = self.sem[eng]
        self.q[eng].append(lambda h, fn=fn, sem=sem: fn(h).then_inc(sem, 1))
        self._post(("e", eng, self.cnt[eng]), eng, R, W)

    def dma(self, q, out, in_, R=(), W=(), owner=None, slow=False):
        R = [_b(x) for x in R]
        W = [_b(x) for x in W]
        owner = _b(owner)
        self._deps(q, R, W)
        so = owner.so
        if so is None or so.owner is not owner:
            so = self.pool[self.pool_i % len(self.pool)]
            self.pool_i += 1
            if so.tot > 0:
                self._wait(q, id(so), so.sem, so.tot)
            so.owner = owner
            owner.so = so
        so.tot += 16
        sem = so.sem
        if slow:
            self.q[q].append(lambda h, out=out, in_=in_, sem=sem: h.dma_start(out=out, in_=in_, allow_slow_non_contiguous=True).then_inc(sem, 16))
        else:
            self.q[q].append(lambda h, out=out, in_=in_, sem=sem: h.dma_start(out=out, in_=in_).then_inc(sem, 16))
        self._post(("d", so, so.tot), ("d", id(so)), R, W)

    def barrier(self):
        for st in self.ENG:
            for e in self.CE:
                if self.cnt[e] > 0 and not (e == st and st == "pe"):
                    self._wait(st, e, self.sem[e], self.cnt[e])
            for so in self.pool:
                if so.tot > 0:
                    self._wait(st, id(so), so.sem, so.tot)

    def mm(self, out, lhsT, rhs, start, stop, R, W):
        self.op("pe", lambda h: h.matmul(out, lhsT=lhsT, rhs=rhs, start=start, stop=stop), R, W)

    def tr(self, out, in_, ident, R, W):
        self.op("pe", lambda h: h.transpose(out, in_, ident), R, W)

    def act(self, out, in_, func, R, W, bias=None, scale=None):
        kw = {}
        if bias is not None:
            kw["bias"] = bias
        if scale is not None:
            kw["scale"] = scale
        self.op("act", lambda h: h.activation(out=out, in_=in_, func=func, **kw), R, W)

    def tt(self, eng, out, in0, in1, op, R, W):
        self.op(eng, lambda h: h.tensor_tensor(out=out, in0=in0, in1=in1, op=op), R, W)

    def ts(self, eng, out, in0, s1, s2, op0, op1, R, W):
        if s2 is None:
            self.op(eng, lambda h: h.tensor_scalar(out=out, in0=in0, scalar1=s1, scalar2=None, op0=op0), R, W)
        else:
            self.op(eng, lambda h: h.tensor_scalar(out=out, in0=in0, scalar1=s1, scalar2=s2, op0=op0, op1=op1), R, W)

    def stt(self, out, in0, scalar, in1, op0, op1, R, W):
        self.op("dve", lambda h: h.scalar_tensor_tensor(out=out, in0=in0, scalar=scalar, in1=in1, op0=op0, op1=op1), R, W)

    def copy(self, eng, out, in_, R, W):
        if eng == "act":
            self.op("act", lambda h: h.copy(out=out, in_=in_), R, W)
        else:
            self.op(eng, lambda h: h.tensor_copy(out=out, in_=in_), R, W)

    def memset(self, eng, ap, val, W):
        self.op(eng, lambda h: h.memset(ap, val), (), W)

    def recip(self, out, in_, R, W):
        self.op("dve", lambda h: h.reciprocal(out=out, in_=in_), R, W)

    def scan(self, out, d0, d1, init, op0, op1, R, W):
        self.op("dve", lambda h: h.tensor_tensor_scan(out=out, data0=d0, data1=d1, initial=init, op0=op0, op1=op1), R, W)

    def emit(self):
        nc = self.nc
        self.barrier()
        q = self.q
        with nc.Block() as block:
            @block.tensor
            def _(h):
                for f in q["pe"]:
                    f(h)

            @block.scalar
            def _(h):
                for f in q["act"]:
                    f(h)

            @block.vector
            def _(h):
                for f in q["dve"]:
                    f(h)

            @block.gpsimd
            def _(h):
                for f in q["pool"]:
                    f(h)

            @block.sync
            def _(h):
                for f in q["sp"]:
                    f(h)


class K:
    def __init__(self, nlayers=DEPTH, dbg=(), ncores=NCORE):
        self.ncores = ncores
        self.nlayers = nlayers
        self.dbg = set(dbg)
        nc = bass.Bass("TRN2", target_bir_lowering=False)
        self.nc = nc
        self.P = Prog(nc)
        self.decl_io()

    def inp(self, name, shape):
        return self.nc.dram_tensor(name, list(shape), F32, kind="ExternalInput").ap()

    def scratch(self, name, shape, dtype):
        kind = "ExternalOutput" if name in self.dbg else "Internal"
        return self.nc.dram_tensor(name, list(shape), dtype, kind=kind).ap()

    def decl_io(self):
        L = DEPTH
        self.xT = self.inp("xT", [D, XTOK])
        self.ctxT = self.inp("ctxT", [D, CTX])
        self.cc = self.inp("cc", [D, 2])
        if self.ncores == 1:
            self.w_mod = self.inp("w_mod", [L, D, 6 * D])
        else:
            self.w_mod = self.inp("w_mod_s", [L, D, 1536])
            self.MI = self.nc.dram_tensor("mod_in", [128, 96], F32)
            self.MO = self.nc.dram_tensor("mod_out", [NCORE * 128, 96], F32)
            self.MIb, self.MOb = Buf("MI"), Buf("MO")
        self.b_mod = self.inp("b_mod", [L, 6 * D])
        self.norm1_g = self.inp("norm1_g", [L, D])
        self.w_in = self.inp("w_in", [L, D, IN_W])
        self.b_in = self.inp("b_in", [L, IN_W])
        self.w_out = self.inp("w_out", [L, D, D])
        self.norm2_g = self.inp("norm2_g", [L, D])
        self.w_ffn_in = self.inp("w_ffn_in", [L, D, 2 * D_FF])
        self.w_ffn_out = self.inp("w_ffn_out", [L, D_FF, D])
        self.norm_f_g = self.inp("norm_f_g", [D])
        self.conv_dw_w = self.inp("conv_dw_w", [L, 31, 512])
        self.conv_dw_b = self.inp("conv_dw_b", [L, 512])
        self.conv_ln_g = self.inp("conv_ln_g", [L, 512])
        self.conv_ln_b = self.inp("conv_ln_b", [L, 512])
        self.ml_norm_g = self.inp("ml_norm_g", [L, 1024])
        self.s5_lam_re = self.inp("s5_lam_re", [L, 2, 32, 64])
        self.s5_lam_im = self.inp("s5_lam_im", [L, 2, 32, 64])
        self.s5_log_step = self.inp("s5_log_step", [L, 2, 32, 64])
        self.s5_b_re = self.inp("s5_b_re", [L, 2, 32, 64, 16])
        self.s5_b_im = self.inp("s5_b_im", [L, 2, 32, 64, 16])
        self.s5_c_re = self.inp("s5_c_re", [L, 2, 32, 16, 64])
        self.s5_c_im = self.inp("s5_c_im", [L, 2, 32, 16, 64])
        self.s5_d = self.inp("s5_d", [L, 512])
        self.s5_w_glu = self.inp("s5_w_glu", [L, 512, 512])
        self.s5_b_glu = self.inp("s5_b_glu", [L, 512])
        self.c16 = self.inp("c16", [16, 24])
        self.c128 = self.inp("c128", [128, 5, 128])
        self.c4 = self.inp("c4", [4, 4, 128])
        self.cflags = self.inp("cflags", [128, 16])
        self.outT = self.nc.dram_tensor("outT", [D, XTOK], F32, kind="ExternalOutput").ap()
        self.XT = self.scratch("XT", [KT, 128, T], F32)
        self.XTb = {(kt, bi): Buf("XT%d_%d" % (kt, bi)) for kt in range(KT) for bi in range(len(TB))}
        self.MIXT = self.scratch("MIXT", [KT, 128, T], BF16)
        self.MIXTb = Buf("MIXT")
        self.ZU = self.scratch("ZU", [4, 128, T], BF16)
        self.ZC = self.scratch("ZC", [8, 128, T], F32)
        self.ZQ = self.scratch("ZQ", [8, 128, T], BF16)
        self.ZK = self.scratch("ZK", [8, 128, T], BF16)
        self.ZG = self.scratch("ZG", [16, T], F32)
        self.KTOK = self.scratch("KTOK", [T, 1024], BF16)
        self.VTOK = self.scratch("VTOK", [T, 1024], BF16)
        self.OTOK = self.scratch("OTOK", [T, 1024], F32)
        self.HH = self.scratch("HH", [32, 128, 2, T], F32)
        self.HHb = Buf("HH")
        self.PLF = 8 * 514 + 8 + 128
        self.XIs = [self.nc.dram_tensor("xchg_in%d" % i, [128, self.PLF], F32) for i in range(DEPTH)]
        self.XOs = [self.nc.dram_tensor("xchg_out%d" % i, [NCORE * 128, self.PLF], F32) for i in range(DEPTH)]
        self.XI, self.XO = self.XIs[0], self.XOs[0]
        self.XIb = Buf("XI")
        self.SCD = self.scratch("SCD", [128, 8, 514], F32)
        self.SCDb = Buf("SCD")
        self.XOb = Buf("XO")
        self.ZTb = Buf("ZT")

    def dump(self, name, tl, shape, dtype=F32):
        if name not in self.dbg:
            return
        d = self.nc.dram_tensor(name, list(shape), dtype, kind="ExternalOutput").ap()
        self.P.dma("sp", d, tl[:], [tl], (), owner=tl)
        self.P.barrier()

    def setup_consts(self):
        P = self.P
        self.ones_bf = P.tile([128, 128], BF16, "ones_bf")
        P.memset("dve", self.ones_bf[:], 1.0, [self.ones_bf])
        self.ones_f = P.tile([128, 128], F32, "ones_f")
        P.memset("dve", self.ones_f[:], 1.0, [self.ones_f])
        self.eps_c = P.tile([128, 2], F32, "eps")
        P.memset("dve", self.eps_c[:], EPS, [self.eps_c])
        self.lneps_c = P.tile([128, 2], F32, "lneps")
        P.memset("dve", self.lneps_c[:], LN_EPS, [self.lneps_c])
        self.one_c = P.tile([128, 2], F32, "one_c")
        P.memset("dve", self.one_c[:], 1.0, [self.one_c])
        ccT = P.tile([128, KT, 2], F32, "ccT")
        P.dma("sp", ccT[:], self.cc.rearrange("(kt p) j -> p kt j", p=128), (), [ccT], owner=ccT, slow=True)
        self.scT = P.tile([128, KT, 2], BF16, "scT")
        P.act(self.scT[:], ccT[:], AF.Silu, [ccT], [self.scT])
        self.wslots = None
        self.wsi = 0
        self.mod = P.tile([128, 6 * KT, 2, 2], F32, "mod")
        self.gm1 = P.tile([128, KT, 2], F32, "gm1")
        self.gm2 = P.tile([128, KT, 2], F32, "gm2")
        self.modg = P.tile([128, NCORE, 96], F32, "modg")

    def new_wslots(self):
        self.wslots = [self.P.tile([128, KT, 512], BF16, "wslot%d" % i) for i in range(3)]

    def wslot(self):
        s = self.wslots[self.wsi % len(self.wslots)]
        self.wsi += 1
        return s

    def load_w(self, src, r0, nk, c0, nc_):
        s = self.wslot()
        self.P.dma("pool", s[:, 0:nk, 0:nc_],
                   src[r0:r0 + nk * 128, c0:c0 + nc_].rearrange("(kt p) c -> p kt c", p=128),
                   (), [s], owner=s)
        return s

    def stage_mod_all(self):
        P = self.P
        m0 = P.mark()
        self.new_wslots()
        ps = P.ps()
        for l in range(DEPTH):
            for cb in range(3):
                w = self.load_w(self.w_mod[l], 0, KT, cb * 512, 512)
                for jj in range(4):
                    j = l * 12 + cb * 4 + jj
                    for kt in range(KT):
                        P.mm(ps[:, 2 * j:2 * j + 2], w[:, kt, jj * 128:(jj + 1) * 128], self.scT[:, kt, :],
                             kt == 0, kt == KT - 1, [w, self.scT], [ps])
        mp = P.tile([128, 96], F32, "modpart")
        P.copy("dve", mp[:], ps[:, 0:96], [ps], [mp])
        P.dma("sp", self.MI.ap()[:, :], mp[:], [mp], [self.MIb], owner=mp)
        mi, mo = self.MI, self.MO
        P.op("pool", lambda h: h.collective_compute("AllGather", ALU.bypass, replica_groups=[list(range(NCORE))],
                                                    ins=[mi.ap().opt()], outs=[mo.ap().opt()]), [self.MIb], [self.MOb])
        P.dma("sp", self.modg[:], self.MO.ap().rearrange("(c p) f -> p c f", p=128), [self.MOb], [self.modg], owner=self.modg)
        P.barrier()
        P.release(m0)

    def stage_mod_finish(self, l):
        P = self.P
        m0 = P.mark()
        bm = P.tile([128, 6 * KT], F32, "bmodT")
        P.dma("sp", bm[:], self.b_mod[l].rearrange("(j p) -> p j", p=128), (), [bm], owner=bm, slow=True)
        n1 = P.tile([128, KT], F32, "n1g")
        P.dma("sp", n1[:], self.norm1_g[l].rearrange("(j p) -> p j", p=128), (), [n1], owner=n1, slow=True)
        n2 = P.tile([128, KT], F32, "n2g")
        P.dma("sp", n2[:], self.norm2_g[l].rearrange("(j p) -> p j", p=128), (), [n2], owner=n2, slow=True)
        mod = self.mod
        src = self.modg[:].rearrange("p c (l j w) -> p c l j w", l=DEPTH, j=12)[:, :, l, :, :]
        dst = mod[:, :, :, 0].rearrange("p (c j) w -> p c j w", c=NCORE)
        bmv = bm[:].rearrange("p (c j) -> p c j", c=NCORE)
        for w_ in range(2):
            P.tt("dve", dst[:, :, :, w_], src[:, :, :, w_], bmv, ALU.add, [self.modg, bm], [mod])
        for (gm, ng, v) in ((self.gm1, n1, 1), (self.gm2, n2, 4)):
            P.ts("dve", gm[:], mod[:, v * KT:(v + 1) * KT, :, 0], 1.0, None, ALU.add, None, [mod], [gm])
            P.tt("dve", gm[:], gm[:], ng[:].unsqueeze(2).to_broadcast([128, KT, 2]), ALU.mult, [gm, ng], [gm])
        P.barrier()
        P.release(m0)

    def stage_mod(self, l):
        P = self.P
        m0 = P.mark()
        self.new_wslots()
        bm = P.tile([128, 6 * KT], F32, "bmodT")
        P.dma("sp", bm[:], self.b_mod[l].rearrange("(j p) -> p j", p=128), (), [bm], owner=bm, slow=True)
        n1 = P.tile([128, KT], F32, "n1g")
        P.dma("sp", n1[:], self.norm1_g[l].rearrange("(j p) -> p j", p=128), (), [n1], owner=n1, slow=True)
        n2 = P.tile([128, KT], F32, "n2g")
        P.dma("sp", n2[:], self.norm2_g[l].rearrange("(j p) -> p j", p=128), (), [n2], owner=n2, slow=True)
        ps = P.ps()
        for cb in range(24):
            w = self.load_w(self.w_mod[l], 0, KT, cb * 512, 512)
            for jj in range(4):
                j = cb * 4 + jj
                for kt in range(KT):
                    P.mm(ps[:, 2 * j:2 * j + 2], w[:, kt, jj * 128:(jj + 1) * 128], self.scT[:, kt, :],
                         kt == 0, kt == KT - 1, [w, self.scT], [ps])
        mod = self.mod
        P.tt("dve", mod[:, :, :, 0], ps[:, 0:192].rearrange("p (j w) -> p j w", w=2),
             bm[:].unsqueeze(2).to_broadcast([128, 6 * KT, 2]), ALU.add, [ps, bm], [mod])
        for (gm, ng, v) in ((self.gm1, n1, 1), (self.gm2, n2, 4)):
            P.ts("dve", gm[:], mod[:, v * KT:(v + 1) * KT, :, 0], 1.0, None, ALU.add, None, [mod], [gm])
            P.tt("dve", gm[:], gm[:], ng[:].unsqueeze(2).to_broadcast([128, KT, 2]), ALU.mult, [gm, ng], [gm])
        P.barrier()
        self.dump("d_mod%d" % l, mod, [128, 6 * KT, 2, 2])
        P.release(m0)

    def stage_load_x(self):
        P = self.P
        m0 = P.mark()
        for kt in range(KT):
            st = P.tile([128, T], F32, "xld")
            P.dma("sp", st[:, 0:CTX], self.ctxT[kt * 128:(kt + 1) * 128, :], (), [st], owner=st)
            P.dma("sp", st[:, CTX:T], self.xT[kt * 128:(kt + 1) * 128, :], (), [st], owner=st)
            bufs = [self.XTb[(kt, bi)] for bi in range(len(TB))]
            P.dma("sp", self.XT[kt], st[:], [st], bufs, owner=st)
            if kt % 4 == 3:
                P.barrier()
                P.release(m0)
        P.barrier()
        P.release(m0)

    def rsqrt(self, out, src, n, scale, eps_t, R):
        P = self.P
        m0 = P.mark()
        v = P.tile([128, n], F32, "rs_v")
        t = P.tile([128, n], F32, "rs_t")
        P.ts("dve", v[:], src, scale, eps_t[:, 0:1], ALU.mult, ALU.add, list(R) + [eps_t], [v])
        P.act(out[:, 0:n], v[:], AF.Sqrt, [v], [out])
        P.recip(out[:, 0:n], out[:, 0:n], [out], [out])
        P.tt("dve", t[:], v[:], out[:, 0:n], ALU.mult, [v, out], [t])
        P.tt("dve", t[:], t[:], out[:, 0:n], ALU.mult, [t, out], [t])
        P.ts("dve", t[:], t[:], -0.5, 1.5, ALU.mult, ALU.add, [t], [t])
        P.tt("dve", out[:, 0:n], out[:, 0:n], t[:], ALU.mult, [out, t], [out])
        P.release(m0)

    def stage_norm(self, hT, gm, shift_v):
        P = self.P
        m0 = P.mark()
        for bi, (t0, n, wh) in enumerate(TB):
            m1 = P.mark()
            xt = P.tile([128, KT, n], F32, "nx")
            for kt in range(KT):
                P.dma("sp" if kt % 2 == 0 else "act", xt[:, kt, :], self.XT[kt, :, t0:t0 + n], [self.XTb[(kt, bi)]], [xt], owner=xt)
            sq = P.tile([128, KT, n], BF16, "nsq")
            P.act(sq[:], xt[:], AF.Square, [xt], [sq])
            ps = P.ps()
            for kt in range(KT):
                P.mm(ps[:, 0:n], self.ones_bf[:], sq[:, kt, :], kt == 0, kt == KT - 1, [sq, self.ones_bf], [ps])
            rstd = P.tile([128, n], F32, "rstd")
            self.rsqrt(rstd, ps[:, 0:n], n, 1.0 / D, self.eps_c, [ps])
            tmp = [P.tile([128, n], F32, "ntmp%d" % i) for i in range(2)]
            for kt in range(KT):
                tp = tmp[kt % 2]
                P.stt(tp[:], xt[:, kt, :], gm[:, kt, wh:wh + 1], rstd[:], ALU.mult, ALU.mult, [xt, gm, rstd], [tp])
                P.act(hT[:, kt, t0:t0 + n], tp[:], AF.Identity, [tp, self.mod], [hT],
                      bias=self.mod[:, shift_v * KT + kt, wh, 0:1])
            P.release(m1)
        P.release(m0)

    def proj_T(self, hT, nk, wsrc, r0, c0, ncols, evac, pre=None):
        P = self.P
        ncb = (ncols + 511) // 512
        for cb in range(ncb):
            cw = min(512, ncols - cb * 512)
            w = self.load_w(wsrc, r0, nk, c0 + cb * 512, cw)
            for jj in range((cw + 127) // 128):
                mw = min(128, cw - jj * 128)
                for bi, (t0, n, wh) in enumerate(TB):
                    if pre is not None:
                        pre(cb * 4 + jj, bi)
                    ps = P.ps()
                    for kt in range(nk):
                        P.mm(ps[0:mw, 0:n], w[:, kt, jj * 128:jj * 128 + mw], hT[:, kt, t0:t0 + n],
                             kt == 0, kt == nk - 1, [w, hT], [ps])
                    evac(cb * 4 + jj, bi, ps)

    def stage_proj_in(self, l, hT):
        P = self.P
        m0 = P.mark()
        binT = P.tile([128, 44], F32, "binT")
        P.dma("sp", binT[:], self.b_in[l, 0:5632].rearrange("(j p) -> p j", p=128), (), [binT], owner=binT, slow=True)
        bing = P.tile([16, 1], F32, "bing")
        P.dma("sp", bing[:], self.b_in[l, 5632:5648].rearrange("(p o) -> p o", o=1), (), [bing], owner=bing, slow=True)
        stg = {}

        def staging(key, dtype):
            if key not in stg:
                stg[key] = {"i": 0, "t": [P.tile([128, 512], dtype, "stg") for _ in range(3)]}
            lst = stg[key]
            s = lst["t"][lst["i"] % 3]
            lst["i"] += 1
            return s

        def evac(j, bi, ps):
            t0, n, wh = TB[bi]
            if j < 4:
                dst, idx, dt, sc = self.ZU, j, BF16, 1.0
            elif j < 12:
                dst, idx, dt, sc = self.ZC, j - 4, F32, 1.0
            elif j < 20:
                dst, idx, dt, sc = self.ZQ, j - 12, BF16, 1.0
            else:
                dst, idx, dt, sc = self.ZK, j - 20, BF16, 1.0 / 16.0
            s = staging(dt, dt)
            if sc == 1.0:
                P.ts("dve", s[:, 0:n], ps[:, 0:n], binT[:, j:j + 1], None, ALU.add, None, [ps, binT], [s])
            else:
                P.ts("dve", s[:, 0:n], ps[:, 0:n], binT[:, j:j + 1], sc, ALU.add, ALU.mult, [ps, binT], [s])
            P.dma("sp", dst[idx, :, t0:t0 + n], s[:, 0:n], [s], [self.ZTb], owner=s)

        self.proj_T(hT, KT, self.w_in[l], 0, 0, 28 * 128, evac)

        def evac_g(j, bi, ps):
            t0, n, wh = TB[bi]
            s = staging(F32, F32)
            P.ts("dve", s[0:16, 0:n], ps[0:16, 0:n], bing[:, 0:1], None, ALU.add, None, [ps, bing], [s])
            P.dma("sp", self.ZG[:, t0:t0 + n], s[0:16, 0:n], [s], [self.ZTb], owner=s)

        self.proj_T(hT, KT, self.w_in[l], 0, 5632, 16, evac_g)
        P.barrier()
        P.release(m0)


    def stage_proj_tok(self, l, hT):
        P = self.P
        m0 = P.mark()
        bb = P.tile([128, 3072], F32, "bkvo")
        P.dma("sp", bb[:], self.b_in[l, 2560:5632].partition_broadcast(128), (), [bb], owner=bb, slow=True)
        stg_b = [P.tile([128, 512], BF16, "stb%d" % i) for i in range(3)]
        stg_f = [P.tile([128, 512], F32, "stf%d" % i) for i in range(3)]
        cnt = 0
        for cb in range(6):
            w = self.load_w(self.w_in[l], 0, KT, 2560 + cb * 512, 512)
            for i in range(NTILE):
                ps = P.ps()
                for kt in range(KT):
                    P.mm(ps[:, 0:512], hT[:, kt, i * 128:(i + 1) * 128], w[:, kt, 0:512], kt == 0, kt == KT - 1, [w, hT], [ps])
                bsl = bb[:, cb * 512:(cb + 1) * 512]
                kind = cb // 2
                c0 = (cb % 2) * 512
                cnt += 1
                if kind == 0:
                    sb = stg_b[cnt % 3]
                    sf = stg_f[cnt % 3]
                    P.tt("dve", sf[:], ps[:, 0:512], bsl, ALU.add, [ps, bb], [sf])
                    P.ts("dve", sb[:], sf[:], 1.0 / 16.0, None, ALU.mult, None, [sf], [sb])
                    P.dma("sp", self.KTOK[i * 128:(i + 1) * 128, c0:c0 + 512], sb[:], [sb], [self.ZTb], owner=sb)
                elif kind == 1:
                    sb = stg_b[cnt % 3]
                    P.tt("dve", sb[:], ps[:, 0:512], bsl, ALU.add, [ps, bb], [sb])
                    P.dma("sp", self.VTOK[i * 128:(i + 1) * 128, c0:c0 + 512], sb[:], [sb], [self.ZTb], owner=sb)
                else:
                    sf = stg_f[cnt % 3]
                    P.tt("dve", sf[:], ps[:, 0:512], bsl, ALU.add, [ps, bb], [sf])
                    P.act(sf[:], sf[:], AF.Sigmoid, [sf], [sf])
                    P.dma("sp", self.OTOK[i * 128:(i + 1) * 128, c0:c0 + 512], sf[:], [sf], [self.ZTb], owner=sf)
        P.barrier()
        P.release(m0)

    def stage_conv(self, l):
        P = self.P
        m0 = P.mark()
        dw = P.tile([128, 4, 32], F32, "dw")
        for ct in range(4):
            P.dma("sp", dw[:, ct, 0:31], self.conv_dw_w[l][:, ct * 128:(ct + 1) * 128].rearrange("k p -> p k"), (), [dw], owner=dw, slow=True)
        prm = P.tile([128, 3, 4, 2], F32, "cprm")
        for i, src in enumerate((self.conv_dw_b, self.conv_ln_g, self.conv_ln_b)):
            P.dma("sp", prm[:, i, :, 0], src[l].rearrange("(ct p) -> p ct", p=128), (), [prm], owner=prm, slow=True)
        Y = P.tile([128, 4, T], F32, "convY")
        for ct in range(4):
            m1 = P.mark()
            a = P.tile([128, T], F32, "ca")
            b = P.tile([128, T], F32, "cb")
            P.dma("sp", a[:], self.ZC[ct], [self.ZTb], [a], owner=a)
            P.dma("act", b[:], self.ZC[4 + ct], [self.ZTb], [b], owner=b)
            P.act(b[:], b[:], AF.Sigmoid, [b], [b])
            P.tt("dve", a[:], a[:], b[:], ALU.mult, [a, b], [a])
            y = Y[:, ct, :]
            P.ts("dve", y, a[:], dw[:, ct, 15:16], prm[:, 0, ct, 0:1], ALU.mult, ALU.add, [a, dw, prm], [Y])
            yx = Y[:, ct, CTX:T].rearrange("p (r w) -> p r w", w=64)
            gx = a[:, CTX:T].rearrange("p (r w) -> p r w", w=64)
            for k in range(31):
                o = k - 15
                if o == 0:
                    continue
                d0, d1 = max(0, -o), CTX - max(0, o)
                P.stt(Y[:, ct, d0:d1], a[:, d0 + o:d1 + o], dw[:, ct, k:k + 1], Y[:, ct, d0:d1], ALU.mult, ALU.add, [a, dw, Y], [Y])
                d0, d1 = max(0, -o), 64 - max(0, o)
                P.stt(yx[:, :, d0:d1], gx[:, :, d0 + o:d1 + o], dw[:, ct, k:k + 1], yx[:, :, d0:d1], ALU.mult, ALU.add, [a, dw, Y], [Y])
            P.release(m1)
        Ysq = P.tile([128, 4, T], F32, "convYsq")
        P.act(Ysq[:], Y[:], AF.Square, [Y], [Ysq])
        stg = [P.tile([128, 512], BF16, "cstg%d" % i) for i in range(3)]
        cnt = 0
        for bi, (t0, n, wh) in enumerate(TB):
            m1 = P.mark()
            ps_s = P.ps()
            ps_q = P.ps()
            for ct in range(4):
                P.mm(ps_s[:, 0:n], self.ones_f[:], Y[:, ct, t0:t0 + n], ct == 0, ct == 3, [Y, self.ones_f], [ps_s])
            for ct in range(4):
                P.mm(ps_q[:, 0:n], self.ones_f[:], Ysq[:, ct, t0:t0 + n], ct == 0, ct == 3, [Ysq, self.ones_f], [ps_q])
            mean = P.tile([128, n], F32, "cmean")
            P.ts("dve", mean[:], ps_s[:, 0:n], 1.0 / 512.0, None, ALU.mult, None, [ps_s], [mean])
            var = P.tile([128, n], F32, "cvar")
            P.tt("dve", var[:], mean[:], mean[:], ALU.mult, [mean], [var])
            P.stt(var[:], ps_q[:, 0:n], 1.0 / 512.0, var[:], ALU.mult, ALU.subtract, [ps_q, var], [var])
            rstd = P.tile([128, n], F32, "crstd")
            self.rsqrt(rstd, var[:], n, 1.0, self.lneps_c, [var])
            tmp = [P.tile([128, n], F32, "ctmp%d" % i) for i in range(2)]
            for ct in range(4):
                tp = tmp[ct % 2]
                P.tt("dve", tp[:], Y[:, ct, t0:t0 + n], mean[:], ALU.subtract, [Y, mean], [tp])
                P.tt("dve", tp[:], tp[:], rstd[:], ALU.mult, [tp, rstd], [tp])
                sb = stg[cnt % 3]
                cnt += 1
                P.act(sb[:, 0:n], tp[:], AF.Silu, [tp, prm], [sb], bias=prm[:, 2, ct, 0:1], scale=prm[:, 1, ct, 0:1])
                P.dma("sp", self.MIXT[4 + ct, :, t0:t0 + n], sb[:, 0:n], [sb], [self.MIXTb], owner=sb)
            P.release(m1)
        P.barrier()
        P.release(m0)


    def s5_setup(self, l, need_B=True):
        P = self.P
        S = {}
        TWO_PI = 2.0 * np.pi
        c128 = P.tile([128, 5, 128], F32, "c128s")
        P.dma("sp", c128[:], self.c128, (), [c128], owner=c128)
        identf = c128[:, 2, :]
        cosT = P.tile([128, 32, 128], F32, "cosT")
        sinT = P.tile([128, 32, 128], F32, "sinT")
        LB = P.tile([128, 2, 2, 16, 128], BF16, "LB") if need_B else None
        CEX = P.tile([128, 2, 2, 16, 128], BF16, "CEX")
        prm = P.tile([128, 8, 32], F32, "s5prm")
        dsk = P.tile([128, 4, 2], F32, "dskip")
        P.dma("sp", dsk[:, :, 0], self.s5_d[l].rearrange("(j p) -> p j", p=128), (), [dsk], owner=dsk, slow=True)
        S.update(cosT=cosT, sinT=sinT, LB=LB, CEX=CEX, prm=prm, dsk=dsk)
        m1 = P.mark()
        lr = P.tile([128, 32], F32, "lr")
        li = P.tile([128, 32], F32, "li")
        ls = P.tile([128, 32], F32, "ls")
        for d in range(2):
            for (t_, src) in ((lr, self.s5_lam_re), (li, self.s5_lam_im), (ls, self.s5_log_step)):
                P.dma("sp", t_[:, d * 16:(d + 1) * 16], src[l, d].rearrange("(gp two) p -> (two p) gp", two=2), (), [t_], owner=t_, slow=True)
        w = [P.tile([128, 32], F32, "s5w%d" % i) for i in range(10)]
        dl, ar, ai, r, th, cs, sn, t0_, t1_, t2_ = w
        P.act(dl[:], ls[:], AF.Exp, [ls], [dl])
        P.tt("dve", ar[:], lr[:], dl[:], ALU.mult, [lr, dl], [ar])
        P.tt("dve", ai[:], li[:], dl[:], ALU.mult, [li, dl], [ai])
        P.act(prm[:, 0, :], ar[:], AF.Exp, [ar], [prm])
        ki = P.tile([128, 32], mybir.dt.int32, "s5ki")
        P.ts("dve", t0_[:], ai[:], 1.0 / TWO_PI, 0.5, ALU.mult, ALU.add, [ai], [t0_])
        P.copy("dve", ki[:], t0_[:], [t0_], [ki])
        P.copy("dve", t0_[:], ki[:], [ki], [t0_])
        P.stt(th[:], t0_[:], -TWO_PI, ai[:], ALU.mult, ALU.add, [t0_, ai], [th])

        def sincos(dst_s, dst_c, ang):
            for (dst, shift) in ((dst_s, 0.0), (dst_c, np.pi / 2)):
                P.ts("dve", t1_[:], ang, float(shift), None, ALU.add, None, [th], [t1_])
                for _ in range(2):
                    P.ts("dve", t2_[:], t1_[:], -float(np.pi), None, ALU.is_lt, None, [t1_], [t2_])
                    P.stt(t1_[:], t2_[:], float(TWO_PI), t1_[:], ALU.mult, ALU.add, [t2_, t1_], [t1_])
                    P.ts("dve", t2_[:], t1_[:], float(np.pi), None, ALU.is_gt, None, [t1_], [t2_])
                    P.stt(t1_[:], t2_[:], -float(TWO_PI), t1_[:], ALU.mult, ALU.add, [t2_, t1_], [t1_])
                P.ts("dve", t2_[:], t1_[:], 3.1415925, -3.1415925, ALU.min, ALU.max, [t1_], [t2_])
                P.act(dst, t2_[:], AF.Sin, [t2_], [cs, sn, prm])

        sincos(sn[:], cs[:], th[:])
        nr = P.tile([128, 32], F32, "s5nr")
        ni = P.tile([128, 32], F32, "s5ni")
        P.tt("dve", nr[:], prm[:, 0, :], cs[:], ALU.mult, [prm, cs], [nr])
        P.ts("dve", nr[:], nr[:], -1.0, None, ALU.add, None, [nr], [nr])
        P.tt("dve", ni[:], prm[:, 0, :], sn[:], ALU.mult, [prm, sn], [ni])
        inv = P.tile([128, 32], F32, "s5inv")
        P.tt("dve", inv[:], lr[:], lr[:], ALU.mult, [lr], [inv])
        P.tt("dve", t0_[:], li[:], li[:], ALU.mult, [li], [t0_])
        P.tt("dve", inv[:], inv[:], t0_[:], ALU.add, [inv, t0_], [inv])
        P.recip(inv[:], inv[:], [inv], [inv])
        kr = P.tile([128, 32], F32, "s5kr")
        kim = P.tile([128, 32], F32, "s5kim")
        P.tt("dve", kr[:], nr[:], lr[:], ALU.mult, [nr, lr], [kr])
        P.tt("dve", t0_[:], ni[:], li[:], ALU.mult, [ni, li], [t0_])
        P.tt("dve", kr[:], kr[:], t0_[:], ALU.add, [kr, t0_], [kr])
        P.tt("dve", kr[:], kr[:], inv[:], ALU.mult, [kr, inv], [kr])
        P.tt("dve", kim[:], ni[:], lr[:], ALU.mult, [ni, lr], [kim])
        P.tt("dve", t0_[:], nr[:], li[:], ALU.mult, [nr, li], [t0_])
        P.tt("dve", kim[:], kim[:], t0_[:], ALU.subtract, [kim, t0_], [kim])
        P.tt("dve", kim[:], kim[:], inv[:], ALU.mult, [kim, inv], [kim])
        if need_B:
            Br = P.tile([128, 32, 16], F32, "s5Br")
            Bi = P.tile([128, 32, 16], F32, "s5Bi")
            for d in range(2):
                P.dma("sp", Br[:, d * 16:(d + 1) * 16, :], self.s5_b_re[l, d].rearrange("(gp two) p c -> (two p) gp c", two=2), (), [Br], owner=Br, slow=True)
                P.dma("act", Bi[:, d * 16:(d + 1) * 16, :], self.s5_b_im[l, d].rearrange("(gp two) p c -> (two p) gp c", two=2), (), [Bi], owner=Bi, slow=True)
            Bbr = P.tile([128, 32, 16], F32, "s5Bbr")
            Bbi = P.tile([128, 32, 16], F32, "s5Bbi")
            tb = P.tile([128, 32, 16], F32, "s5tb")
            krb = kr[:].unsqueeze(2).to_broadcast([128, 32, 16])
            kib = kim[:].unsqueeze(2).to_broadcast([128, 32, 16])
            P.tt("dve", Bbr[:], Br[:], krb, ALU.mult, [Br, kr], [Bbr])
            P.tt("dve", tb[:], Bi[:], kib, ALU.mult, [Bi, kim], [tb])
            P.tt("dve", Bbr[:], Bbr[:], tb[:], ALU.subtract, [Bbr, tb], [Bbr])
            P.tt("dve", Bbi[:], Bi[:], krb, ALU.mult, [Bi, kr], [Bbi])
            P.tt("dve", tb[:], Br[:], kib, ALU.mult, [Br, kim], [tb])
            P.tt("dve", Bbi[:], Bbi[:], tb[:], ALU.add, [Bbi, tb], [Bbi])
            BX = P.tile([128, 2, 2, 16, 128], F32, "s5BX")
            P.memset("dve", BX[:], 0.0, [BX])
            for d in range(2):
                for ri, src in enumerate((Bbr, Bbi)):
                    for two in range(2):
                        ps_ = slice(two * 64, (two + 1) * 64)
                        for q in range(4):
                            dst = BX[ps_, d, ri, :, q * 32 + two * 16:q * 32 + two * 16 + 16].rearrange("p (g q) c -> p g q c", q=4)[:, :, q, :]
                            srcv = src[ps_, d * 16:(d + 1) * 16, :].rearrange("p (g q) c -> p g q c", q=4)[:, :, q, :]
                            P.copy("dve", dst, srcv, [src], [BX])
            for d in range(2):
                for ri in range(2):
                    for gp in range(16):
                        ps = P.ps()
                        P.mm(ps[:, 0:128], BX[:, d, ri, gp, :], identf, True, True, [BX, c128], [ps])
                        P.copy("act", LB[:, d, ri, gp, :], ps[:, 0:128], [ps], [LB])
        Cr = P.tile([128, 32, 16], F32, "s5Cr")
        Ci = P.tile([128, 32, 16], F32, "s5Ci")
        for d in range(2):
            for gp in range(16):
                for two in range(2):
                    P.dma("sp", Cr[two * 64:(two + 1) * 64, d * 16 + gp, :], self.s5_c_re[l, d, 2 * gp + two].rearrange("c p -> p c"), (), [Cr], owner=Cr, slow=True)
                    P.dma("act", Ci[two * 64:(two + 1) * 64, d * 16 + gp, :], self.s5_c_im[l, d, 2 * gp + two].rearrange("c p -> p c"), (), [Ci], owner=Ci, slow=True)
        P.ts("dve", Ci[:], Ci[:], -1.0, None, ALU.mult, None, [Ci], [Ci])
        P.memset("dve", CEX[:], 0.0, [CEX])
        for d in range(2):
            for ri, src in enumerate((Cr, Ci)):
                for two in range(2):
                    ps_ = slice(two * 64, (two + 1) * 64)
                    for q in range(4):
                        dst = CEX[ps_, d, ri, :, q * 32 + two * 16:q * 32 + two * 16 + 16].rearrange("p (g q) c -> p g q c", q=4)[:, :, q, :]
                        srcv = src[ps_, d * 16:(d + 1) * 16, :].rearrange("p (g q) c -> p g q c", q=4)[:, :, q, :]
                        P.copy("dve", dst, srcv, [src], [CEX])
        P.memset("dve", cosT[:, :, 0:1], 1.0, [cosT])
        P.memset("dve", sinT[:, :, 0:1], 0.0, [sinT])
        P.copy("dve", prm[:, 4, :], cs[:], [cs], [prm])
        P.copy("dve", prm[:, 5, :], sn[:], [sn], [prm])
        P.ts("dve", prm[:, 6, :], sn[:], -1.0, None, ALU.mult, None, [sn], [prm])
        tA = P.tile([128, 32, 64], F32, "s5tA")
        tB = P.tile([128, 32, 64], F32, "s5tB")
        for k in range(7):
            m = 1 << k
            cb_ = cs[:].unsqueeze(2).to_broadcast([128, 32, m])
            sb_ = sn[:].unsqueeze(2).to_broadcast([128, 32, m])
            P.tt("dve", tA[:, :, 0:m], cosT[:, :, 0:m], cb_, ALU.mult, [cosT, cs], [tA])
            P.tt("dve", tB[:, :, 0:m], sinT[:, :, 0:m], sb_, ALU.mult, [sinT, sn], [tB])
            P.tt("dve", cosT[:, :, m:2 * m], tA[:, :, 0:m], tB[:, :, 0:m], ALU.subtract, [tA, tB], [cosT])
            P.tt("dve", tA[:, :, 0:m], cosT[:, :, 0:m], sb_, ALU.mult, [cosT, sn], [tA])
            P.tt("dve", tB[:, :, 0:m], sinT[:, :, 0:m], cb_, ALU.mult, [sinT, cs], [tB])
            P.tt("dve", sinT[:, :, m:2 * m], tA[:, :, 0:m], tB[:, :, 0:m], ALU.add, [tA, tB], [sinT])
            P.tt("dve", t0_[:], cs[:], cs[:], ALU.mult, [cs], [t0_])
            P.tt("dve", t1_[:], sn[:], sn[:], ALU.mult, [sn], [t1_])
            P.tt("dve", t2_[:], cs[:], sn[:], ALU.mult, [cs, sn], [t2_])
            P.tt("dve", cs[:], t0_[:], t1_[:], ALU.subtract, [t0_, t1_], [cs])
            P.ts("dve", sn[:], t2_[:], 2.0, None, ALU.mult, None, [t2_], [sn])
        P.copy("dve", prm[:, 1, :], cs[:], [cs], [prm])
        P.copy("dve", prm[:, 2, :], sn[:], [sn], [prm])
        P.ts("dve", prm[:, 3, :], sn[:], -1.0, None, ALU.mult, None, [sn], [prm])
        P.release(m1)
        return S

    def s5_more_setup(self, S):
        P = self.P
        prm = S["prm"]
        RP = P.tile([128, 32, 128], F32, "s5RP")
        ex = P.tile([128, 8, 32], F32, "s5ex")
        S["RP"], S["ex"] = RP, ex
        m1 = P.mark()
        rr = P.tile([128, 32], F32, "s5rr")
        P.copy("dve", rr[:], prm[:, 0, :], [prm], [rr])
        P.copy("dve", RP[:, :, 0], prm[:, 0, :], [prm], [RP])
        for k in range(7):
            m = 1 << k
            P.tt("dve", RP[:, :, m:2 * m], RP[:, :, 0:m], rr[:].unsqueeze(2).to_broadcast([128, 32, m]), ALU.mult, [RP, rr], [RP])
            P.tt("dve", rr[:], rr[:], rr[:], ALU.mult, [rr], [rr])
        P.tt("dve", ex[:, 0, :], rr[:], prm[:, 1, :], ALU.mult, [rr, prm], [ex])
        P.tt("dve", ex[:, 1, :], rr[:], prm[:, 2, :], ALU.mult, [rr, prm], [ex])
        P.ts("dve", ex[:, 2, :], ex[:, 1, :], -1.0, None, ALU.mult, None, [ex], [ex])
        lr_ = P.tile([128, 32], F32, "s5Lr")
        li_ = P.tile([128, 32], F32, "s5Li")
        ta = P.tile([128, 32], F32, "s5Lta")
        tb = P.tile([128, 32], F32, "s5Ltb")
        P.copy("dve", lr_[:], ex[:, 0, :], [ex], [lr_])
        P.copy("dve", li_[:], ex[:, 1, :], [ex], [li_])
        for k in range(3):
            P.tt("dve", ta[:], lr_[:], lr_[:], ALU.mult, [lr_], [ta])
            P.tt("dve", tb[:], li_[:], li_[:], ALU.mult, [li_], [tb])
            P.tt("dve", li_[:], lr_[:], li_[:], ALU.mult, [lr_, li_], [li_])
            P.ts("dve", li_[:], li_[:], 2.0, None, ALU.mult, None, [li_], [li_])
            P.tt("dve", lr_[:], ta[:], tb[:], ALU.subtract, [ta, tb], [lr_])
        P.copy("dve", ex[:, 3, :], lr_[:], [lr_], [ex])
        P.copy("dve", ex[:, 4, :], li_[:], [li_], [ex])
        P.ts("dve", ex[:, 5, :], li_[:], -1.0, None, ALU.mult, None, [li_], [ex])
        P.release(m1)

    def stage_s5A(self, l):
        P = self.P
        m0 = P.mark()
        S = self.s5_setup(l)
        cosT, sinT, LB, prm = S["cosT"], S["sinT"], S["LB"], S["prm"]
        ST = P.tile([128, 32, 4], F32, "S5ST")
        self.S5ST = ST
        uT = P.tile([128, 4, T], BF16, "s5uT")
        for j in range(4):
            P.dma("sp", uT[:, j, :], self.ZU[j], [self.ZTb], [uT], owner=uT)
        ghs = [P.tile([128, 2, T], F32, "s5gh%d" % i) for i in range(2)]
        hhs = [P.tile([128, 2, T], F32, "s5hh%d" % i) for i in range(2)]
        tmps = [P.tile([128, 512], F32, "s5tm%d" % i) for i in range(8)]
        gcnt = [0]
        ini = [P.tile([128, 4], F32, "s5ini%d" % i) for i in range(2)]
        inic = 0
        it4 = P.tile([128, 4], F32, "s5it4")

        def rv(ap3, d):
            return ap3 if d == 0 else ap3[:, :, ::-1]

        cnt = 0
        for gp in range(16):
            G4 = gp // 4
            for d in range(2):
                col = d * 16 + gp
                gh = ghs[cnt % 2]
                hh_ = hhs[cnt % 2]
                cnt += 1
                ctab = cosT[:, col, :]
                stab = sinT[:, col, :]
                for (t0, n, wh) in TB:
                    nb = n // 128
                    pr = P.ps()
                    pi = P.ps()
                    P.mm(pr[:, 0:n], LB[:, d, 0, gp, :], uT[:, G4, t0:t0 + n], True, True, [LB, uT], [pr])
                    P.mm(pi[:, 0:n], LB[:, d, 1, gp, :], uT[:, G4, t0:t0 + n], True, True, [LB, uT], [pi])
                    cb3 = ctab.unsqueeze(1).to_broadcast([128, nb, 128])
                    sb3 = stab.unsqueeze(1).to_broadcast([128, nb, 128])
                    prv = rv(pr[:, 0:n].rearrange("p (b j) -> p b j", j=128), d)
                    piv = rv(pi[:, 0:n].rearrange("p (b j) -> p b j", j=128), d)
                    ts_ = tmps[4 * (gcnt[0] % 2):4 * (gcnt[0] % 2) + 4]
                    gcnt[0] += 1
                    tv = [rv(tm[:, 0:n].rearrange("p (b j) -> p b j", j=128), d) for tm in ts_]
                    gre = rv(gh[:, 0, t0:t0 + n].rearrange("p (b j) -> p b j", j=128), d)
                    gim = rv(gh[:, 1, t0:t0 + n].rearrange("p (b j) -> p b j", j=128), d)
                    P.tt("dve", tv[0], prv, cb3, ALU.mult, [pr, cosT], [ts_[0]])
                    P.tt("dve", tv[1], piv, sb3, ALU.mult, [pi, sinT], [ts_[1]])
                    P.tt("dve", tv[2], piv, cb3, ALU.mult, [pi, cosT], [ts_[2]])
                    P.tt("dve", tv[3], prv, sb3, ALU.mult, [pr, sinT], [ts_[3]])
                    P.tt("pool", gre, tv[0], tv[1], ALU.add, [ts_[0], ts_[1]], [gh])
                    P.tt("pool", gim, tv[2], tv[3], ALU.subtract, [ts_[2], ts_[3]], [gh])
                rb = prm[:, 0, col:col + 1].to_broadcast([128, 128])
                segs = ([0, 1], list(range(2, NTILE))) if d == 0 else ([1, 0], list(range(NTILE - 1, 1, -1)))
                for si, order in enumerate(segs):
                    prev = None
                    for bi_ in order:
                        a = bi_ * 128
                        if d == 0:
                            gsl = lambda t_, ri, a=a: t_[:, ri, a:a + 128]
                            last = lambda t_, ri, a=a: t_[:, ri, a + 127:a + 128]
                        else:
                            gsl = lambda t_, ri, a=a: t_[:, ri, a:a + 128][:, ::-1]
                            last = lambda t_, ri, a=a: t_[:, ri, a:a + 1]
                        Rx = [prm, gh]
                        if prev is None:
                            i_re, i_im = 0.0, 0.0
                        else:
                            it = ini[inic % 2]
                            inic += 1
                            P.ts("dve", it[:, 2:3], prev[0], prm[:, 1, col:col + 1], None, ALU.mult, None, [hh_, prm], [it])
                            P.stt(it[:, 0:1], prev[1], prm[:, 3, col:col + 1], it[:, 2:3], ALU.mult, ALU.add, [hh_, prm, it], [it])
                            P.ts("dve", it[:, 3:4], prev[1], prm[:, 1, col:col + 1], None, ALU.mult, None, [hh_, prm], [it])
                            P.stt(it[:, 1:2], prev[0], prm[:, 2, col:col + 1], it[:, 3:4], ALU.mult, ALU.add, [hh_, prm, it], [it])
                            i_re, i_im = it[:, 0:1], it[:, 1:2]
                            Rx = [prm, gh, it]
                        P.scan(gsl(hh_, 0), rb, gsl(gh, 0), i_re, ALU.mult, ALU.add, Rx, [hh_])
                        P.scan(gsl(hh_, 1), rb, gsl(gh, 1), i_im, ALU.mult, ALU.add, Rx, [hh_])
                        prev = (last(hh_, 0), last(hh_, 1))
                    c127 = cosT[:, col, 127:128]
                    s127 = sinT[:, col, 127:128]
                    P.tt("dve", it4[:, 0:1], prev[0], c127, ALU.mult, [hh_, cosT], [it4])
                    P.tt("dve", it4[:, 1:2], prev[1], s127, ALU.mult, [hh_, sinT], [it4])
                    P.tt("dve", it4[:, 2:3], prev[0], s127, ALU.mult, [hh_, sinT], [it4])
                    P.tt("dve", it4[:, 3:4], prev[1], c127, ALU.mult, [hh_, cosT], [it4])
                    P.tt("dve", ST[:, col, 2 * si:2 * si + 1], it4[:, 0:1], it4[:, 1:2], ALU.subtract, [it4], [ST])
                    P.tt("dve", ST[:, col, 2 * si + 1:2 * si + 2], it4[:, 2:3], it4[:, 3:4], ALU.add, [it4], [ST])
                P.dma("sp", self.HH[col], hh_[:], [hh_], [self.HHb], owner=hh_)
        P.dma("sp", self.XI.ap()[:, 4120:4248], ST[:].rearrange("p c f -> p (c f)"), [ST], [self.XIb], owner=ST)
        P.barrier()
        P.release(m0)

    def stage_s5B(self, l):
        P = self.P
        m0 = P.mark()
        S = self.s5_setup(l, need_B=False)
        self.s5_more_setup(S)
        TinS5 = P.tile([128, 32, 2], F32, "TinS5")
        XG = P.tile([128, NCORE, 128], F32, "s5XG")
        P.dma("sp", XG[:], self.XO.ap()[:, 4120:4248].rearrange("(j p) f -> p j f", p=128), [self.XOb], [XG], owner=XG)
        self.s5_combine(l, S, XG, TinS5)
        cosT, sinT, CEX, prm, dsk, RP, ex = S["cosT"], S["sinT"], S["CEX"], S["prm"], S["dsk"], S["RP"], S["ex"]
        uT = P.tile([128, 4, T], BF16, "s5uT")
        for j in range(4):
            P.dma("sp", uT[:, j, :], self.ZU[j], [self.ZTb], [uT], owner=uT)
        gT = P.tile([128, 4, T], BF16, "s5gT")
        gF = P.tile([128, 4, T], F32, "s5gF")
        mg = P.mark()
        Hall = P.tile([128, 8, 2, T], BF16, "s5H")
        hhs = [P.tile([128, 2, T], F32, "s5hh%d" % i) for i in range(2)]
        tmps = [P.tile([128, 512], F32, "s5tm%d" % i) for i in range(8)]
        gcnt = [0]
        chs = [P.tile([128, 8], F32, "s5ch%d" % i) for i in range(2)]
        chi = 0

        def rv(ap3, d):
            return ap3 if d == 0 else ap3[:, :, ::-1]

        cnt = 0
        for G4 in range(4):
            for q in range(4):
                gp = G4 * 4 + q
                for d in range(2):
                    col = d * 16 + gp
                    hh_ = hhs[cnt % 2]
                    cnt += 1
                    P.dma("sp", hh_[:], self.HH[col], [self.HHb], [hh_], owner=hh_)
                    ctab = cosT[:, col, :]
                    stab = sinT[:, col, :]
                    ch = chs[chi % 2]
                    chi += 1
                    P.ts("dve", ch[:, 2:3], TinS5[:, col, 0:1], prm[:, 4, col:col + 1], None, ALU.mult, None, [TinS5, prm], [ch])
                    P.stt(ch[:, 0:1], TinS5[:, col, 1:2], prm[:, 6, col:col + 1], ch[:, 2:3], ALU.mult, ALU.add, [TinS5, prm, ch], [ch])
                    P.ts("dve", ch[:, 3:4], TinS5[:, col, 1:2], prm[:, 4, col:col + 1], None, ALU.mult, None, [TinS5, prm], [ch])
                    P.stt(ch[:, 1:2], TinS5[:, col, 0:1], prm[:, 5, col:col + 1], ch[:, 3:4], ALU.mult, ALU.add, [TinS5, prm, ch], [ch])
                    order = list(range(2, NTILE)) if d == 0 else list(range(NTILE - 1, 1, -1))
                    cur = (0, 1)
                    for bi_ in order:
                        a = bi_ * 128
                        for ri in range(2):
                            v = hh_[:, ri, a:a + 128] if d == 0 else hh_[:, ri, a:a + 128][:, ::-1]
                            P.stt(v, RP[:, col, :], ch[:, cur[ri]:cur[ri] + 1], v, ALU.mult, ALU.add, [RP, ch, hh_], [hh_])
                        nxt = (4, 5) if cur == (0, 1) else (0, 1)
                        P.ts("dve", ch[:, 6:7], ch[:, cur[0]:cur[0] + 1], ex[:, 0, col:col + 1], None, ALU.mult, None, [ch, ex], [ch])
                        P.stt(ch[:, nxt[0]:nxt[0] + 1], ch[:, cur[1]:cur[1] + 1], ex[:, 2, col:col + 1], ch[:, 6:7], ALU.mult, ALU.add, [ch, ex], [ch])
                        P.ts("dve", ch[:, 7:8], ch[:, cur[1]:cur[1] + 1], ex[:, 0, col:col + 1], None, ALU.mult, None, [ch, ex], [ch])
                        P.stt(ch[:, nxt[1]:nxt[1] + 1], ch[:, cur[0]:cur[0] + 1], ex[:, 1, col:col + 1], ch[:, 7:8], ALU.mult, ALU.add, [ch, ex], [ch])
                        cur = nxt
                    for (t0, n, wh) in TB:
                        nb = n // 128
                        cb3 = ctab.unsqueeze(1).to_broadcast([128, nb, 128])
                        sb3 = stab.unsqueeze(1).to_broadcast([128, nb, 128])
                        hre = rv(hh_[:, 0, t0:t0 + n].rearrange("p (b j) -> p b j", j=128), d)
                        him = rv(hh_[:, 1, t0:t0 + n].rearrange("p (b j) -> p b j", j=128), d)
                        ts_ = tmps[4 * (gcnt[0] % 2):4 * (gcnt[0] % 2) + 4]
                        gcnt[0] += 1
                        tv = [rv(tm[:, 0:n].rearrange("p (b j) -> p b j", j=128), d) for tm in ts_]
                        ore = rv(Hall[:, q * 2 + d, 0, t0:t0 + n].rearrange("p (b j) -> p b j", j=128), d)
                        oim = rv(Hall[:, q * 2 + d, 1, t0:t0 + n].rearrange("p (b j) -> p b j", j=128), d)
                        P.tt("dve", tv[0], hre, cb3, ALU.mult, [hh_, cosT], [ts_[0]])
                        P.tt("dve", tv[1], him, sb3, ALU.mult, [hh_, sinT], [ts_[1]])
                        P.tt("dve", tv[2], hre, sb3, ALU.mult, [hh_, sinT], [ts_[2]])
                        P.tt("dve", tv[3], him, cb3, ALU.mult, [hh_, cosT], [ts_[3]])
                        P.tt("pool", ore, tv[0], tv[1], ALU.subtract, [ts_[0], ts_[1]], [Hall])
                        P.tt("pool", oim, tv[2], tv[3], ALU.add, [ts_[2], ts_[3]], [Hall])
            for (t0, n, wh) in TB:
                py = P.ps()
                k = 0
                for q in range(4):
                    for d in range(2):
                        for ri in range(2):
                            P.mm(py[:, 0:n], CEX[:, d, ri, G4 * 4 + q, :], Hall[:, q * 2 + d, ri, t0:t0 + n], k == 0, k == 15, [CEX, Hall], [py])
                            k += 1
                yt = tmps[0]
                P.stt(yt[:, 0:n], uT[:, G4, t0:t0 + n], dsk[:, G4, 0:1], py[:, 0:n], ALU.mult, ALU.add, [uT, dsk, py], [yt])
                P.act(gF[:, G4, t0:t0 + n], yt[:, 0:n], AF.Gelu, [yt], [gF])
                P.copy("pool", gT[:, G4, t0:t0 + n], gF[:, G4, t0:t0 + n], [gF], [gT])
        P.release(mg)
        self.new_wslots_small()
        bgl = P.tile([128, 4, 2], F32, "bglu")
        P.dma("sp", bgl[:, :, 0], self.s5_b_glu[l].rearrange("(j p) -> p j", p=128), (), [bgl], owner=bgl, slow=True)
        stg = [P.tile([128, 512], BF16, "s5stg%d" % i) for i in range(2)]
        sgt = [P.tile([128, 512], F32, "s5sg%d" % i) for i in range(2)]
        cnt2 = [0]

        def evac(j, bi, ps):
            t0, n, wh = TB[bi]
            sg = sgt[cnt2[0] % 2]
            sb = stg[cnt2[0] % 2]
            cnt2[0] += 1
            P.act(sg[:, 0:n], ps[:, 0:n], AF.Sigmoid, [ps, bgl], [sg], bias=bgl[:, j, 0:1])
            P.tt("dve", sb[:, 0:n], sg[:, 0:n], gF[:, j, t0:t0 + n], ALU.mult, [sg, gF], [sb])
            P.dma("sp", self.MIXT[j, :, t0:t0 + n], sb[:, 0:n], [sb], [self.MIXTb], owner=sb)

        self.proj_T(gT, 4, self.s5_w_glu[l], 0, 0, 512, evac)
        P.barrier()
        P.release(m0)

    def s5_combine(self, l, S, XG, TinS5):
        P = self.P
        ex = S["ex"]
        fl = P.tile([128, 16], F32, "s5flags")
        P.dma("sp", fl[:], self.cflags, (), [fl], owner=fl)
        m1 = P.mark()
        STl = P.tile([128, 32, 4], F32, "s5STl")
        P.dma("sp", STl[:].rearrange("p c f -> p (c f)"), self.XI.ap()[:, 4120:4248], [self.XIb], [STl], owner=STl)
        tr = P.tile([128, 16], F32, "s5c_tr")
        ti = P.tile([128, 16], F32, "s5c_ti")
        na = P.tile([128, 16], F32, "s5c_na")
        nb_ = P.tile([128, 16], F32, "s5c_nb")
        for d in range(2):
            cs_ = slice(d * 16, (d + 1) * 16)
            P.copy("dve", tr[:], STl[:, cs_, 0], [STl], [tr])
            P.copy("dve", ti[:], STl[:, cs_, 1], [STl], [ti])
            Lr, Li = ex[:, 3, cs_], ex[:, 4, cs_]
            for j in (range(NCORE) if d == 0 else range(NCORE - 1, -1, -1)):
                xg = XG[:, j, :].rearrange("p (c f) -> p c f", f=4)
                hr, hi = xg[:, cs_, 2], xg[:, cs_, 3]
                f = fl[:, d * 8 + j:d * 8 + j + 1]
                P.tt("dve", na[:], tr[:], Lr, ALU.mult, [tr, ex], [na])
                P.tt("dve", nb_[:], ti[:], Li, ALU.mult, [ti, ex], [nb_])
                P.tt("dve", na[:], na[:], nb_[:], ALU.subtract, [na, nb_], [na])
                P.tt("dve", na[:], na[:], hr, ALU.add, [na, XG], [na])
                P.tt("dve", nb_[:], tr[:], Li, ALU.mult, [tr, ex], [nb_])
                P.tt("dve", na[:], na[:], tr[:], ALU.subtract, [na, tr], [na])
                P.stt(tr[:], na[:], f, tr[:], ALU.mult, ALU.add, [na, fl, tr], [tr])
                P.tt("dve", na[:], ti[:], Lr, ALU.mult, [ti, ex], [na])
                P.tt("dve", na[:], na[:], nb_[:], ALU.add, [na, nb_], [na])
                P.tt("dve", na[:], na[:], hi, ALU.add, [na, XG], [na])
                P.tt("dve", na[:], na[:], ti[:], ALU.subtract, [na, ti], [na])
                P.stt(ti[:], na[:], f, ti[:], ALU.mult, ALU.add, [na, fl, ti], [ti])
            P.copy("dve", TinS5[:, cs_, 0], tr[:], [tr], [TinS5])
            P.copy("dve", TinS5[:, cs_, 1], ti[:], [ti], [TinS5])
        P.release(m1)

    def new_wslots_small(self):
        self.wslots = [self.P.tile([128, 4, 512], BF16, "wslotS%d" % i) for i in range(2)]

    def ml_setup(self, l):
        P = self.P
        S = {}
        c16 = P.tile([16, 24], F32, "c16")
        P.dma("sp", c16[:], self.c16, (), [c16], owner=c16)
        c4 = P.tile([4, 4, 128], F32, "c4")
        P.dma("sp", c4[:], self.c4, (), [c4], owner=c4)
        c128 = P.tile([128, 5, 128], F32, "c128")
        P.dma("sp", c128[:], self.c128, (), [c128], owner=c128)
        S["c4"], S["c128"] = c4, c128
        AZ = [P.tile([4, T + 2], F32, "AZ%d" % d) for d in range(2)]
        AT = P.tile([128, NTILE, 8, 2], F32, "AT")
        BT = P.tile([128, NTILE, 8, 2], F32, "BT")
        S["REFZ"] = P.tile([128, 8, 11, 2], F32, "REFZ")
        m1 = P.mark()
        G = P.tile([16, T], F32, "mlG")
        P.dma("sp", G[:], self.ZG, [self.ZTb], [G], owner=G)
        L1 = P.tile([16, T], F32, "mlL1")
        P.act(L1[:], G[:], AF.Exp, [G], [L1], scale=-1.0)
        P.act(L1[:], L1[:], AF.Ln, [L1, self.one_c], [L1], bias=self.one_c[0:16, 0:1])
        one16 = P.tile([16, T], F32, "one16")
        P.memset("dve", one16[:], 1.0, [one16])
        FZ = P.tile([16, T + 1], F32, "mlFZ")
        P.memset("dve", FZ[:, 0:1], 0.0, [FZ])
        P.scan(FZ[:, 1:T + 1], one16[:], L1[:], 0.0, ALU.mult, ALU.add, [one16, L1], [FZ])
        BE = [P.tile([4, T], F32, "BE%d" % d) for d in range(2)]
        P.memset("dve", AZ[0][:], 0.0, [AZ[0]])
        P.memset("dve", AZ[1][:], 0.0, [AZ[1]])
        blocks = [(0, 512), (512, 512), (1024, 257)]
        for (u0, n) in blocks:
            ps = P.ps()
            P.mm(ps[0:4, 0:n], c16[:, 12:16], FZ[:, u0:u0 + n], True, True, [c16, FZ], [ps])
            P.copy("dve", AZ[0][:, u0:u0 + n], ps[0:4, 0:n], [ps], [AZ[0]])
            ps = P.ps()
            P.mm(ps[0:4, 0:n], c16[:, 16:20], FZ[:, u0:u0 + n], True, True, [c16, FZ], [ps])
            P.copy("dve", AZ[1][:, 1 + u0:1 + u0 + n], ps[0:4, 0:n], [ps], [AZ[1]])
        for (u0, n) in [(0, 512), (512, 512), (1024, 256)]:
            ps = P.ps()
            P.mm(ps[0:4, 0:n], c16[:, 0:4], G[:, u0:u0 + n], True, False, [c16, G], [ps])
            P.mm(ps[0:4, 0:n], c16[:, 8:12], FZ[:, 1 + u0:1 + u0 + n], False, True, [c16, FZ], [ps])
            P.copy("dve", BE[0][:, u0:u0 + n], ps[0:4, 0:n], [ps], [BE[0]])
            ps = P.ps()
            P.mm(ps[0:4, 0:n], c16[:, 4:8], G[:, u0:u0 + n], True, False, [c16, G], [ps])
            P.mm(ps[0:4, 0:n], c16[:, 20:24], FZ[:, u0:u0 + n], False, True, [c16, FZ], [ps])
            P.copy("dve", BE[1][:, u0:u0 + n], ps[0:4, 0:n], [ps], [BE[1]])
        ident4 = c128[0:4, 2, 0:4]
        for d in range(2):
            for i in range(NTILE):
                ps = P.ps()
                P.mm(ps[:, 0:4], AZ[d][:, 1 + i * 128:1 + (i + 1) * 128], ident4, True, True, [AZ[d], c128], [ps])
                P.mm(ps[:, 4:8], BE[d][:, i * 128:(i + 1) * 128], ident4, True, True, [BE[d], c128], [ps])
                P.copy("dve", AT[:, i, d * 4:(d + 1) * 4, 0], ps[:, 0:4], [ps], [AT])
                P.copy("dve", BT[:, i, d * 4:(d + 1) * 4, 0], ps[:, 4:8], [ps], [BT])
        REFZ = S["REFZ"]
        for d in range(2):
            for h in range(4):
                c = d * 4 + h
                for (u0, n, i0, ni) in [(0, 512, 0, 4), (512, 512, 4, 4), (1024, 258, 8, 2)]:
                    ps = P.ps()
                    P.mm(ps[:, 0:n], c4[:, h, :], AZ[d][:, u0:u0 + n], True, True, [c4, AZ[d]], [ps])
                    P.copy("dve", REFZ[:, c, i0:i0 + ni, :], ps[:, 0:ni * 128].rearrange("p (i r) -> p i r", r=128)[:, :, 0:2], [ps], [REFZ])
                    if u0 == 1024:
                        P.copy("dve", REFZ[:, c, 10, :], ps[:, 256:258], [ps], [REFZ])
        S["AZ"], S["AT"], S["BT"] = AZ, AT, BT
        P.release(m1)
        return S

    def ml_chunk(self, S, d, h, i, main):
        P = self.P
        c = d * 4 + h
        a = i * 128
        AT, BT, REFZ, AZ, c4, c128 = S["AT"], S["BT"], S["REFZ"], S["AZ"], S["c4"], S["c128"]
        ZQs, ZKs, Vx, HS = S["ZQs"], S["ZKs"], S["Vx"], S["HS"]
        Cst, Cb = S["Cst"][c], S["Cb"][c]
        Kt = S["Ktc"][i]
        if d == 0:
            z_in, z_e, z1, z2 = REFZ[:, c, i, 0:1], REFZ[:, c, i + 1, 0:1], REFZ[:, c, i, 0:1], REFZ[:, c, i + 1, 0:1]
        else:
            z_in, z_e, z1, z2 = REFZ[:, c, i + 1, 1:2], REFZ[:, c, i, 1:2], REFZ[:, c, i + 1, 1:2], REFZ[:, c, i, 1:2]
        k_ = S["sci"]
        S["sci"] += 1
        sc = S["sc"][k_ % 8]
        P.tt("dve", sc[:, 0:1], AT[:, i, c, 0:1], z_in, ALU.subtract, [AT, REFZ], [sc])
        P.tt("dve", sc[:, 1:2], BT[:, i, c, 0:1], z_e, ALU.add, [BT, REFZ], [sc])
        P.tt("dve", sc[:, 2:3], z2, z1, ALU.subtract, [REFZ], [sc])
        P.act(sc[:, 4:7], sc[:, 0:3], AF.Exp, [sc], [sc])
        wj, es, dec = sc[:, 4:5], sc[:, 5:6], sc[:, 6:7]
        if main:
            ps_d = P.ps()
            P.mm(ps_d[:, 0:128], c4[:, h, :], AZ[d][:, a + 1:a + 129], True, True, [c4, AZ[d]], [ps_d])
            ps_st = P.ps()
            for kt in range(2):
                P.mm(ps_st[:, 0:128], ZKs[:, 2 * h + kt, a:a + 128], ZQs[:, 2 * h + kt, a:a + 128], kt == 0, kt == 1, [ZKs, ZQs], [ps_st])
            Dt = S["Dt"][k_ % 4]
            P.act(Dt[:], ps_d[:, 0:128], AF.Exp, [ps_d, BT], [Dt], bias=BT[:, i, c, 0:1])
            Wt = S["Wt"][k_ % 4]
            P.tt("dve", Dt[:], ps_st[:, 0:128], Dt[:], ALU.mult, [ps_st, Dt], [Dt])
            P.tt("dve", Wt[:], Dt[:], c128[:, 3 + d, :], ALU.mult, [Dt, c128], [Wt])
            ps_o = P.ps()
            P.mm(ps_o[:, 0:257], Wt[:], Vx[:, i, h, :], True, True, [Wt, Vx], [ps_o])
            ps_i = P.ps()
            for kt in range(2):
                P.mm(ps_i[:, 0:257], ZQs[:, 2 * h + kt, a:a + 128], Cb[:, kt, :], kt == 0, kt == 1, [ZQs, Cb], [ps_i])
            tmp = S["tmp"][k_ % 4]
            P.copy("act", tmp[:], ps_o[:, 0:257], [ps_o], [tmp])
            P.stt(tmp[:], ps_i[:, 0:257], wj, tmp[:], ALU.mult, ALU.add, [ps_i, sc, tmp], [tmp])
            P.act(sc[:, 8:9], tmp[:, 256:257], AF.Abs, [tmp], [sc])
            P.ts("dve", sc[:, 8:9], sc[:, 8:9], 1.0, None, ALU.max, None, [sc], [sc])
            P.recip(sc[:, 9:10], sc[:, 8:9], [sc], [sc])
            hsl = HS[:, i, h * 256:(h + 1) * 256]
            P.stt(hsl, tmp[:, 0:256], sc[:, 9:10], hsl, ALU.mult, ALU.add, [tmp, sc, S["HSb"][i][h]], [S["HSb"][i][h]])
        kw = S["kw"][k_ % 4]
        P.ts("dve", kw[:], Kt[:, h * 256:(h + 1) * 256], es, None, ALU.mult, None, [Kt, sc], [kw])
        for kt in range(2):
            ps_s = P.ps()
            P.mm(ps_s[:, 0:257], kw[:, kt * 128:(kt + 1) * 128], Vx[:, i, h, :], True, True, [kw, Vx], [ps_s])
            P.stt(Cst[:, kt, :], Cst[:, kt, :], dec, ps_s[:, 0:257], ALU.mult, ALU.add, [Cst, sc, ps_s], [Cst])
        P.copy("act", Cb[:], Cst[:], [Cst], [Cb])

    def ml_steps(self, S, pairs, main):
        P = self.P
        if os.environ.get("ML_SEQ"):
            for d in range(2):
                for h in range(4):
                    for (fi, bi_) in pairs:
                        i = fi if d == 0 else bi_
                        kt_ = S["Ktbuf"][S["Kti"] % len(S["Ktbuf"])]
                        S["Kti"] += 1
                        P.dma("sp", kt_[:], self.KTOK[i * 128:(i + 1) * 128, :], [self.ZTb], [kt_], owner=kt_)
                        S["Ktc"][i] = kt_
                        self.ml_chunk(S, d, h, i, main)
            S["Ktc"].clear()
            return
        for (fi, bi_) in pairs:
            for i in sorted(set((fi, bi_))):
                if i not in S["Ktc"]:
                    kt_ = S["Ktbuf"][S["Kti"] % len(S["Ktbuf"])]
                    S["Kti"] += 1
                    P.dma("sp", kt_[:], self.KTOK[i * 128:(i + 1) * 128, :], [self.ZTb], [kt_], owner=kt_)
                    S["Ktc"][i] = kt_
            for h in range(4):
                self.ml_chunk(S, 0, h, fi, main)
                self.ml_chunk(S, 1, h, bi_, main)
            S["Ktc"].clear()

    def stage_exchange(self):
        P = self.P
        if self.ncores == 1:
            for j in range(NCORE):
                P.dma("sp", self.XO.ap()[j * 128:(j + 1) * 128, :], self.XI.ap()[:, :], [self.XIb], [self.XOb], owner=self.XOb)
        else:
            xi, xo = self.XI, self.XO
            P.op("pool", lambda h: h.collective_compute("AllGather", ALU.bypass, replica_groups=[list(range(NCORE))],
                                                        ins=[xi.ap().opt()], outs=[xo.ap().opt()]), [self.XIb], [self.XOb])
        P.barrier()

    def stage_mlstm(self, l):
        P = self.P
        need_ctx = l < DEPTH - 1
        m0 = P.mark()
        S = self.ml_setup(l)
        S["HS"] = P.tile([128, NTILE, 1024], F32, "HS")
        S["HSb"] = [[Buf("HS%d_%d" % (i, h)) for h in range(4)] for i in range(NTILE)]
        for i in range(NTILE):
            P.memset("pool", S["HS"][:, i, :], 0.0, [S["HS"]] + [S["HSb"][i][h] for h in range(4)])
        mph = P.mark()
        S["ZQs"] = P.tile([128, 8, T], BF16, "ZQs")
        S["ZKs"] = P.tile([128, 8, T], BF16, "ZKs")
        for j in range(8):
            P.dma("sp", S["ZQs"][:, j, :], self.ZQ[j], [self.ZTb], [S["ZQs"]], owner=S["ZQs"])
            P.dma("act", S["ZKs"][:, j, :], self.ZK[j], [self.ZTb], [S["ZKs"]], owner=S["ZKs"])
        S["Vx"] = P.tile([128, NTILE, 4, 257], BF16, "Vx")
        P.memset("dve", S["Vx"][:], 1.0, [S["Vx"]])
        for i in range(NTILE):
            P.dma("act", S["Vx"][:, i, :, 0:256], self.VTOK[i * 128:(i + 1) * 128, :].rearrange("p (h v) -> p h v", v=256),
                  [self.ZTb], [S["Vx"]], owner=S["Vx"])
        S["Ktbuf"] = [P.tile([128, 1024], BF16, "Ktb%d" % k) for k in range(4)]
        S["Kti"] = 0
        S["Ktc"] = {}
        S["sc"] = [P.tile([128, 12], F32, "mlsc%d" % k) for k in range(8)]
        S["sci"] = 0
        S["Dt"] = [P.tile([128, 128], F32, "Dt%d" % k) for k in range(4)]
        S["Wt"] = [P.tile([128, 128], BF16, "Wt%d" % k) for k in range(4)]
        S["tmp"] = [P.tile([128, 257], F32, "mltmp%d" % k) for k in range(4)]
        S["kw"] = [P.tile([128, 256], BF16, "kw%d" % k) for k in range(4)]
        S["Cst"] = [P.tile([128, 2, 257], F32, "Cst%d" % c) for c in range(8)]
        S["Cb"] = [P.tile([128, 2, 257], BF16, "Cb%d" % c) for c in range(8)]
        bt = P.tile([128, 8], F32, "mlBtot")
        REFZ = S["REFZ"]
        XI, XO = self.XI.ap(), self.XO.ap()

        def zero_states():
            for c in range(8):
                P.memset("dve", S["Cst"][c][:], 0.0, [S["Cst"][c]])
                P.memset("pool", S["Cb"][c][:], 0.0, [S["Cb"][c]])

        zero_states()
        self.ml_steps(S, [(0, 1), (1, 0)], True)
        for c in range(8):
            P.dma("sp", self.SCD[:, c, :], S["Cst"][c][:].rearrange("p k f -> p (k f)"), [S["Cst"][c]], [self.SCDb], owner=S["Cst"][c])
        zero_states()
        self.ml_steps(S, [(2 + st, NTILE - 1 - st) for st in range(8)], False)
        for c in range(8):
            P.dma("sp", XI[:, c * 514:(c + 1) * 514], S["Cst"][c][:].rearrange("p k f -> p (k f)"), [S["Cst"][c]], [self.XIb], owner=S["Cst"][c])
            if c < 4:
                P.tt("dve", bt[:, c:c + 1], REFZ[:, c, 10, 0:1], REFZ[:, c, 2, 0:1], ALU.subtract, [REFZ], [bt])
            else:
                P.tt("dve", bt[:, c:c + 1], REFZ[:, c, 2, 1:2], REFZ[:, c, 10, 1:2], ALU.subtract, [REFZ], [bt])
        P.dma("sp", XI[:, 4112:4120], bt[:], [bt], [self.XIb], owner=bt)
        self.stage_exchange()
        fl = P.tile([128, 16], F32, "mlflags")
        P.dma("sp", fl[:], self.cflags, (), [fl], owner=fl)
        Bg = P.tile([128, NCORE, 8], F32, "mlBg")
        P.dma("sp", Bg[:], XO[:, 4112:4120].rearrange("(j p) f -> p j f", p=128), [self.XOb], [Bg], owner=Bg)
        Aw = P.tile([128, NCORE, 8], F32, "mlAw")
        P.act(Aw[:], Bg[:], AF.Exp, [Bg], [Aw])
        P.ts("dve", Aw[:], Aw[:], -1.0, None, ALU.add, None, [Aw], [Aw])
        for d in range(2):
            P.tt("dve", Aw[:, :, d * 4:(d + 1) * 4], Aw[:, :, d * 4:(d + 1) * 4],
                 fl[:, d * 8:(d + 1) * 8].unsqueeze(2).to_broadcast([128, NCORE, 4]), ALU.mult, [Aw, fl], [Aw])
        P.ts("dve", Aw[:], Aw[:], 1.0, None, ALU.add, None, [Aw], [Aw])
        sg = [P.tile([128, 514], F32, "mlSG%d" % k) for k in range(3)]
        sgi = 0
        for d in range(2):
            for h in range(4):
                c = d * 4 + h
                Cst, Cb = S["Cst"][c], S["Cb"][c]
                Cf = Cst[:].rearrange("p k f -> p (k f)")
                P.dma("sp", Cf, self.SCD[:, c, :], [self.SCDb], [Cst], owner=Cst)
                for j in (range(NCORE) if d == 0 else range(NCORE - 1, -1, -1)):
                    g_ = sg[sgi % 3]
                    sgi += 1
                    P.dma("act", g_[:], XO[j * 128:(j + 1) * 128, c * 514:(c + 1) * 514], [self.XOb], [g_], owner=g_)
                    P.ts("dve", g_[:], g_[:], fl[:, d * 8 + j:d * 8 + j + 1], None, ALU.mult, None, [g_, fl], [g_])
                    P.stt(Cf, Cf, Aw[:, j, c:c + 1], g_[:], ALU.mult, ALU.add, [Cst, Aw, g_], [Cst])
                P.copy("act", Cb[:], Cst[:], [Cst], [Cb])
        self.ml_steps(S, [(2 + st, NTILE - 1 - st) for st in range(8)], True)
        P.barrier()
        P.release(mph)
        mlg = P.tile([128, 1024], F32, "mlg")
        P.dma("sp", mlg[:], self.ml_norm_g[l].partition_broadcast(128), (), [mlg], owner=mlg, slow=True)
        HS = S["HS"]
        identf = S["c128"][:, 2, :]
        stg = [P.tile([128, 128], BF16, "mstg%d" % k) for k in range(3)]
        cnt = 0
        for i in range(NTILE if need_ctx else NTILE):
            m1 = P.mark()
            ot = P.tile([128, 1024], F32, "mlO")
            P.dma("sp", ot[:], self.OTOK[i * 128:(i + 1) * 128, :], [self.ZTb], [ot], owner=ot)
            hs = HS[:, i, :].rearrange("p (h v) -> p h v", v=256)
            st = P.tile([128, 8], F32, "mlst")
            P.op("dve", lambda hh, st=st, hs=hs: hh.tensor_reduce(out=st[:, 0:4], in_=hs, axis=AX.X, op=ALU.add), [HS], [st])
            P.ts("dve", st[:, 0:4], st[:, 0:4], 1.0 / 256.0, None, ALU.mult, None, [st], [st])
            cen = P.tile([128, 4, 256], F32, "mlcen")
            P.tt("dve", cen[:], hs, st[:, 0:4].unsqueeze(2).to_broadcast([128, 4, 256]), ALU.subtract, [HS, st], [cen])
            sq = P.tile([128, 4, 256], F32, "mlsq")
            P.act(sq[:], cen[:], AF.Square, [cen], [sq])
            P.op("dve", lambda hh, st=st, sq=sq: hh.tensor_reduce(out=st[:, 4:8], in_=sq[:], axis=AX.X, op=ALU.add), [sq], [st])
            rs = P.tile([128, 4], F32, "mlrs")
            self.rsqrt(rs, st[:, 4:8], 4, 1.0 / 256.0, self.lneps_c, [st])
            P.tt("dve", cen[:], cen[:], rs[:].unsqueeze(2).to_broadcast([128, 4, 256]), ALU.mult, [cen, rs], [cen])
            cf = cen[:].rearrange("p h v -> p (h v)")
            P.tt("dve", cf, cf, mlg[:], ALU.mult, [cen, mlg], [cen])
            P.tt("dve", cf, cf, ot[:], ALU.mult, [cen, ot], [cen])
            for cb in range(8):
                ps = P.ps()
                P.mm(ps[:, 0:128], cen[:].rearrange("p h v -> p (h v)")[:, cb * 128:(cb + 1) * 128], identf, True, True, [cen, S["c128"]], [ps])
                sb = stg[cnt % 3]
                cnt += 1
                P.copy("act", sb[:], ps[:, 0:128], [ps], [sb])
                P.dma("sp", self.MIXT[8 + cb, :, i * 128:(i + 1) * 128], sb[:], [sb], [self.MIXTb], owner=sb)
            P.release(m1)
        P.barrier()
        P.release(m0)

    def stage_wout(self, l):
        P = self.P
        m0 = P.mark()
        self.new_wslots()
        mt = P.tile([128, KT, T], BF16, "mixT")
        for kt in range(KT):
            P.dma("sp" if kt % 2 == 0 else "act", mt[:, kt, :], self.MIXT[kt], [self.MIXTb], [mt], owner=mt)
        self.resid_proj(mt, KT, self.w_out[l], 0, 2)
        P.barrier()
        P.release(m0)

    def resid_proj(self, hT, nk, wsrc, r0, gate_v):
        P = self.P
        xs = [P.tile([128, 512], F32, "xrmw%d" % i) for i in range(4)]
        cnt = [0]
        cur = {}

        def pre(j, bi):
            t0, n, wh = TB[bi]
            s = xs[cnt[0] % 4]
            cnt[0] += 1
            cur[(j, bi)] = s
            P.dma("act", s[:, 0:n], self.XT[j, :, t0:t0 + n], [self.XTb[(j, bi)]], [s], owner=s)

        def evac(j, bi, ps):
            t0, n, wh = TB[bi]
            s = cur.pop((j, bi))
            xb = self.XTb[(j, bi)]
            P.stt(s[:, 0:n], ps[:, 0:n], self.mod[:, gate_v * KT + j, wh, 0:1], s[:, 0:n], ALU.mult, ALU.add,
                  [ps, self.mod, s], [s])
            P.dma("sp", self.XT[j, :, t0:t0 + n], s[:, 0:n], [s], [xb], owner=s)

        self.proj_T(hT, nk, wsrc, r0, 0, D, evac, pre)

    def stage_ffn(self, l):
        P = self.P
        m0 = P.mark()
        self.new_wslots()
        h2 = P.tile([128, KT, T], BF16, "h2T")
        self.stage_norm(h2, self.gm2, 3)
        FCH = 1024
        nch = (D_FF + FCH - 1) // FCH
        for ch in range(nch):
            m1 = P.mark()
            f0 = ch * FCH
            fw = min(FCH, D_FF - f0)
            nft = fw // 128
            actT = P.tile([128, nft, T], BF16, "actT")
            gtmp = [P.tile([128, 512], F32, "gtmp%d" % i) for i in range(2)]
            gi = [0]
            for cb in range(fw // 512):
                wg = self.load_w(self.w_ffn_in[l], 0, KT, f0 + cb * 512, 512)
                wu = self.load_w(self.w_ffn_in[l], 0, KT, D_FF + f0 + cb * 512, 512)
                for jj in range(4):
                    ft = cb * 4 + jj
                    for bi, (t0, n, wh) in enumerate(TB):
                        pg = P.ps()
                        pu = P.ps()
                        for kt in range(KT):
                            P.mm(pg[:, 0:n], wg[:, kt, jj * 128:(jj + 1) * 128], h2[:, kt, t0:t0 + n], kt == 0, kt == KT - 1, [wg, h2], [pg])
                        for kt in range(KT):
                            P.mm(pu[:, 0:n], wu[:, kt, jj * 128:(jj + 1) * 128], h2[:, kt, t0:t0 + n], kt == 0, kt == KT - 1, [wu, h2], [pu])
                        g = gtmp[gi[0] % 2]
                        gi[0] += 1
                        P.act(g[:, 0:n], pg[:, 0:n], AF.Silu, [pg], [g])
                        P.tt("dve", actT[:, ft, t0:t0 + n], g[:, 0:n], pu[:, 0:n], ALU.mult, [g, pu], [actT])
            self.resid_proj(actT, nft, self.w_ffn_out[l], f0, 5)
            P.barrier()
            P.release(m1)
        P.release(m0)

    def stage_final(self):
        P = self.P
        m0 = P.mark()
        gf = P.tile([128, KT], F32, "gf")
        P.dma("sp", gf[:], self.norm_f_g.rearrange("(j p) -> p j", p=128), (), [gf], owner=gf, slow=True)
        for bi, (t0, n, wh) in enumerate(TB):
            if wh == 1:
                continue
            m1 = P.mark()
            xt = P.tile([128, KT, n], F32, "fx")
            for kt in range(KT):
                P.dma("sp" if kt % 2 == 0 else "act", xt[:, kt, :], self.XT[kt, :, t0:t0 + n], [self.XTb[(kt, bi)]], [xt], owner=xt)
            sq = P.tile([128, KT, n], BF16, "fsq")
            P.act(sq[:], xt[:], AF.Square, [xt], [sq])
            ps = P.ps()
            for kt in range(KT):
                P.mm(ps[:, 0:n], self.ones_bf[:], sq[:, kt, :], kt == 0, kt == KT - 1, [sq, self.ones_bf], [ps])
            rstd = P.tile([128, n], F32, "frstd")
            self.rsqrt(rstd, ps[:, 0:n], n, 1.0 / D, self.eps_c, [ps])
            ot = P.tile([128, KT, n], F32, "fo")
            for kt in range(KT):
                P.stt(ot[:, kt, :], xt[:, kt, :], gf[:, kt:kt + 1], rstd[:], ALU.mult, ALU.mult, [xt, gf, rstd], [ot])
            for kt in range(KT):
                P.dma("sp" if kt % 2 == 0 else "act", self.outT[kt * 128:(kt + 1) * 128, t0 - CTX:t0 - CTX + n], ot[:, kt, :], [ot], (), owner=ot)
            P.barrier()
            P.release(m1)
        P.release(m0)

    def stage_mix_stub(self):
        P = self.P
        m0 = P.mark()
        z = P.tile([128, T], BF16, "zeros")
        P.memset("dve", z[:], 0.0, [z])
        for kt in range(KT):
            P.dma("sp", self.MIXT[kt], z[:], [z], [self.MIXTb], owner=z)
        P.barrier()
        P.release(m0)

    def build(self, mixers=True):
        P = self.P
        self.setup_consts()
        self.stage_load_x()
        if self.ncores > 1:
            self.stage_mod_all()
        for l in range(self.nlayers):
            if self.ncores > 1:
                self.stage_mod_finish(l)
            else:
                self.stage_mod(l)
            m0 = P.mark()
            self.new_wslots()
            h1 = P.tile([128, KT, T], BF16, "h1T")
            self.stage_norm(h1, self.gm1, 0)
            self.dump("d_h1_%d" % l, h1, [128, KT, T], BF16)
            self.stage_proj_in(l, h1)
            self.stage_proj_tok(l, h1)
            P.release(m0)
            self.XI, self.XO = self.XIs[l], self.XOs[l]
            self.stage_conv(l)
            self.stage_s5A(l)
            self.stage_mlstm(l)
            self.stage_s5B(l)
            self.stage_wout(l)
            self.stage_ffn(l)
        self.stage_final()
        P.emit()
        return self.nc


def host_consts():
    c16 = np.zeros((16, 24), np.float32)
    for m in range(4):
        c16[m, 0 + m] = 1.0
        c16[8 + m, 4 + m] = 1.0
        c16[4 + m, 8 + m] = 1.0
        c16[4 + m, 12 + m] = -1.0
        c16[12 + m, 16 + m] = 1.0
        c16[12 + m, 20 + m] = -1.0
    c128 = np.zeros((128, 5, 128), np.float32)
    sidx = np.arange(128)[:, None]
    jidx = np.arange(128)[None, :]
    c128[:, 0, :] = np.where(sidx <= jidx, 0.0, -30000.0)
    c128[:, 1, :] = np.where(sidx >= jidx, 0.0, -30000.0)
    c128[:, 2, :] = np.eye(128, dtype=np.float32)
    c128[:, 3, :] = (sidx <= jidx).astype(np.float32)
    c128[:, 4, :] = (sidx >= jidx).astype(np.float32)
    c4 = np.zeros((4, 4, 128), np.float32)
    for h in range(4):
        c4[h, h, :] = 1.0
    return {"c16": c16, "c128": c128, "c4": c4}


WKEYS1 = ["w_mod"]
WKEYS = ["s5_lam_re", "s5_lam_im", "s5_log_step", "s5_b_re", "s5_b_im", "s5_c_re", "s5_c_im", "s5_d", "s5_w_glu", "s5_b_glu",
         "conv_dw_w", "conv_dw_b", "conv_ln_g", "conv_ln_b", "ml_norm_g", "b_mod", "norm1_g", "w_in", "b_in", "w_out", "norm2_g", "w_ffn_in", "w_ffn_out", "norm_f_g"]


def make_in_maps(inputs):
    x = np.asarray(inputs["x"], np.float32)[0]
    ctx = np.asarray(inputs["ctx"], np.float32)[0]
    ctxT = np.ascontiguousarray(ctx.T)
    cc = np.ascontiguousarray(np.stack([np.asarray(inputs["c"], np.float32)[0], np.asarray(inputs["c_ctx"], np.float32)], axis=1))
    shared = {k: np.ascontiguousarray(np.asarray(inputs[k], np.float32)) for k in WKEYS}
    shared.update(host_consts())
    ncore = x.shape[0] // XTOK
    w_mod = np.asarray(inputs["w_mod"], np.float32)
    maps = []
    for c in range(x.shape[0] // XTOK):
        m = dict(shared)
        fl = np.zeros((128, 16), np.float32)
        fl[:, 0:8] = (np.arange(8) < c).astype(np.float32)[None, :]
        fl[:, 8:16] = (np.arange(8) > c).astype(np.float32)[None, :]
        m["cflags"] = fl
        if ncore == 1:
            m["w_mod"] = np.ascontiguousarray(w_mod)
        else:
            m["w_mod_s"] = np.ascontiguousarray(w_mod[:, :, c * 1536:(c + 1) * 1536])
        m["xT"] = np.ascontiguousarray(x[c * XTOK:(c + 1) * XTOK].T)
        m["ctxT"] = ctxT
        m["cc"] = cc
        maps.append(m)
    return maps


def kernel(**inputs):
    k = K()
    nc = k.build()
    maps = make_in_maps(inputs)
    res = run_bass_kernel_spmd(nc, maps, core_ids=list(range(NCORE)))
    outs = [np.asarray(r["outT"]).T for r in res.results]
    return np.concatenate(outs, axis=0)[None].astype(np.float32)
```
